# Optimizing a Trainium2 kernel written in Bass

```python
import math
import jax
import jax.numpy as jnp
from jax import lax
import numpy as np

D_MODEL = 4096
BATCH = 4
SEQ = 4096
DEPTH = 1


HEAD_DIM = 128
HGRN_DK = 128
HGRN_DV = 128
N_HGRN_HEADS = (D_MODEL // 2) // HGRN_DV
HGRN_F_DIM = N_HGRN_HEADS * HGRN_DK
HGRN_WIDTH = N_HGRN_HEADS * HGRN_DV
N_MOBA_HEADS = (D_MODEL // 2) // HEAD_DIM
MOBA_WIDTH = N_MOBA_HEADS * HEAD_DIM
D_MIX = HGRN_WIDTH + MOBA_WIDTH
D_IN_PROJ = 2 * HGRN_F_DIM + 2 * HGRN_WIDTH + 3 * MOBA_WIDTH
D_FF = ((8 * D_MODEL // 3 + 255) // 256) * 256
MOBA_BLOCK = 256
MOBA_TOPK = 3
HGRN_CHUNK = 64
Q_CHUNK = 16
MACARON_WEIGHT = 0.5
RMS_EPS = 1e-6

kernel_name = 'hybrid_hgrn2_moba_macaron_layer'


def rmsnorm(x, g):
    xf = x.astype(jnp.float32)
    r = lax.rsqrt(jnp.mean(xf * xf, axis=-1, keepdims=True) + RMS_EPS)
    return (xf * r).astype(x.dtype) * g


def swiglu(h, w_gate, w_up, w_down):
    return (jax.nn.silu(h @ w_gate) * (h @ w_up)) @ w_down


def _heads(a, n_heads):
    b_, s_, w_ = a.shape
    return a.reshape(b_, s_, n_heads, w_ // n_heads).transpose(0, 2, 1, 3)


def _merge(a):
    b_, h_, s_, d_ = a.shape
    return a.transpose(0, 2, 1, 3).reshape(b_, s_, h_ * d_)


def hgrn2_chunkwise(q, k, v, log_f):
    b_, h_, s_, dk = q.shape
    dv = v.shape[-1]
    n_chunks = s_ // HGRN_CHUNK

    def to_chunks(a):
        return jnp.moveaxis(a.reshape(b_, h_, n_chunks, HGRN_CHUNK, a.shape[-1]), 2, 0)

    causal = jnp.tril(jnp.ones((HGRN_CHUNK, HGRN_CHUNK), dtype=bool))

    def step(state, inp):
        qc, kc, vc, gc = inp
        cum = jnp.cumsum(gc, axis=2)
        o_inter = jnp.einsum('bhtk,bhkv->bhtv', qc * jnp.exp(cum), state)
        rel = cum[:, :, :, None, :] - cum[:, :, None, :, :]
        decay = jnp.exp(jnp.where(causal[:, :, None], rel, -jnp.inf))
        scores = jnp.einsum('bhtk,bhsk,bhtsk->bhts', qc, kc, decay)
        o_intra = jnp.einsum('bhts,bhsv->bhtv', scores, vc)
        last = cum[:, :, -1, :]
        k_to_end = kc * jnp.exp(last[:, :, None, :] - cum)
        new_state = jnp.exp(last)[..., None] * state + jnp.einsum('bhsk,bhsv->bhkv', k_to_end, vc)
        return new_state, o_inter + o_intra

    state0 = jnp.zeros((b_, h_, dk, dv), jnp.float32)
    _, o = lax.scan(step, state0, (to_chunks(q), to_chunks(k), to_chunks(v), to_chunks(log_f)))
    return jnp.moveaxis(o, 0, 2).reshape(b_, h_, s_, dv)


def hgrn2_mixer(hq, hf, hi, hg, lower_bound, norm_gain):
    f32 = jnp.float32
    forget = lower_bound + (1.0 - lower_bound) * jax.nn.sigmoid(hf.astype(f32))
    q = _heads(jax.nn.silu(hq.astype(f32)) * HGRN_DK ** -0.5, N_HGRN_HEADS)
    k = _heads(1.0 - forget, N_HGRN_HEADS)
    log_f = _heads(jnp.log(forget), N_HGRN_HEADS)
    v = _heads(hi.astype(f32), N_HGRN_HEADS)
    o = hgrn2_chunkwise(q, k, v, log_f)
    o = rmsnorm(o, norm_gain)
    o = _merge(o) * jax.nn.silu(hg.astype(f32))
    return o.astype(hq.dtype)


def moba_attention(q, k, v):
    f32 = jnp.float32
    b_, h_, s_, d_ = q.shape
    L = MOBA_BLOCK
    nb = -(-s_ // L)
    s_pad = nb * L
    topk = min(MOBA_TOPK, max(nb - 1, 1))
    scale = d_ ** -0.5
    slopes = jnp.exp2(-8.0 * jnp.arange(1, h_ + 1, dtype=f32) / h_)
    pad = ((0, 0), (0, 0), (0, s_pad - s_), (0, 0))
    kb = jnp.pad(k, pad).reshape(b_, h_, nb, L, d_)
    vb = jnp.pad(v, pad).reshape(b_, h_, nb, L, d_)
    k_mean = jnp.mean(kb.astype(f32), axis=3)
    gate = jnp.einsum('bhtd,bhnd->bhtn', q.astype(f32), k_mean)
    q_block = jnp.arange(s_) // L
    past = jnp.arange(nb)[None, :] < q_block[:, None]
    gate = jnp.where(past, gate, -jnp.inf)
    _, sel = lax.top_k(gate, topk)

    n_qc = s_ // Q_CHUNK
    qc_all = jnp.moveaxis(q.reshape(b_, h_, n_qc, Q_CHUNK, d_), 2, 0)
    sel_all = jnp.moveaxis(sel.reshape(b_, h_, n_qc, Q_CHUNK, topk), 2, 0)
    bi = jnp.arange(b_)[:, None, None, None]
    hi = jnp.arange(h_)[None, :, None, None]
    offs = jnp.arange(L)

    def one_chunk(args):
        c, qi, si = args
        t = c * Q_CHUNK + jnp.arange(Q_CHUNK)
        own = (c * Q_CHUNK) // L
        k_own = lax.dynamic_index_in_dim(kb, own, axis=2, keepdims=False)
        v_own = lax.dynamic_index_in_dim(vb, own, axis=2, keepdims=False)
        k_sel = kb[bi, hi, si]
        v_sel = vb[bi, hi, si]
        s_sel = si[..., None] * L + offs
        sc_sel = (jnp.einsum('bhtd,bhtnld->bhtnl', qi, k_sel).astype(f32) * scale
                  - slopes[:, None, None, None] * (t[:, None, None] - s_sel).astype(f32))
        valid = jnp.arange(topk)[None, :] < (t // L)[:, None]
        sc_sel = jnp.where(valid[:, :, None], sc_sel, -jnp.inf)
        s_own = own * L + offs
        sc_own = (jnp.einsum('bhtd,bhsd->bhts', qi, k_own).astype(f32) * scale
                  - slopes[:, None, None] * (t[:, None] - s_own[None, :]).astype(f32))
        sc_own = jnp.where(s_own[None, :] <= t[:, None], sc_own, -jnp.inf)
        scores = jnp.concatenate([sc_sel.reshape(b_, h_, Q_CHUNK, topk * L), sc_own], axis=-1)
        p = jax.nn.softmax(scores, axis=-1)
        p_sel = p[..., :topk * L].reshape(b_, h_, Q_CHUNK, topk, L)
        p_own = p[..., topk * L:]
        o = (jnp.einsum('bhtnl,bhtnld->bhtd', p_sel, v_sel.astype(f32))
             + jnp.einsum('bhts,bhsd->bhtd', p_own, v_own.astype(f32)))
        return o.astype(q.dtype)

    o = lax.map(one_chunk, (jnp.arange(n_qc), qc_all, sel_all))
    return jnp.moveaxis(o, 0, 2).reshape(b_, h_, s_, d_)


def hybrid_mixer(h, w_in, lower_bound, hgrn_norm_gain, w_out):
    proj = h @ w_in
    cuts = np.cumsum([HGRN_F_DIM, HGRN_F_DIM, HGRN_WIDTH, HGRN_WIDTH,
                      MOBA_WIDTH, MOBA_WIDTH]).tolist()
    hq, hf, hi, hg, mq, mk, mv = jnp.split(proj, cuts, axis=-1)
    o_hgrn = hgrn2_mixer(hq, hf, hi, hg, lower_bound, hgrn_norm_gain)
    o_moba = _merge(moba_attention(_heads(mq, N_MOBA_HEADS), _heads(mk, N_MOBA_HEADS),
                                   _heads(mv, N_MOBA_HEADS)))
    return jnp.concatenate([o_hgrn, o_moba], axis=-1) @ w_out


def setup_inputs(seed: int = 0) -> dict:
    key = jax.random.key(seed)
    ks = jax.random.split(key, 16)
    f32 = jnp.float32

    def dense(k, shape):
        return jax.random.normal(k, shape, f32) * shape[-2] ** -0.5

    def gain(k, shape):
        return 1.0 + 0.02 * jax.random.normal(k, shape, f32)

    return {
        'x': jax.random.normal(ks[0], (BATCH, SEQ, D_MODEL), f32),
        'ffn1_norm': gain(ks[1], (DEPTH, D_MODEL)),
        'ffn1_w_gate': dense(ks[2], (DEPTH, D_MODEL, D_FF)),
        'ffn1_w_up': dense(ks[3], (DEPTH, D_MODEL, D_FF)),
        'ffn1_w_down': dense(ks[4], (DEPTH, D_FF, D_MODEL)),
        'mix_norm': gain(ks[5], (DEPTH, D_MODEL)),
        'w_in': dense(ks[6], (DEPTH, D_MODEL, D_IN_PROJ)),
        'hgrn_lower_bounds': 0.1 * jax.random.normal(ks[7], (DEPTH + 1, HGRN_F_DIM), f32),
        'hgrn_out_norm': gain(ks[8], (DEPTH, HGRN_DV)),
        'w_out': dense(ks[9], (DEPTH, D_MIX, D_MODEL)),
        'ffn2_norm': gain(ks[10], (DEPTH, D_MODEL)),
        'ffn2_w_gate': dense(ks[11], (DEPTH, D_MODEL, D_FF)),
        'ffn2_w_up': dense(ks[12], (DEPTH, D_MODEL, D_FF)),
        'ffn2_w_down': dense(ks[13], (DEPTH, D_FF, D_MODEL)),
        'final_norm': gain(ks[14], (D_MODEL,)),
    }


def reference(x, ffn1_norm, ffn1_w_gate, ffn1_w_up, ffn1_w_down, mix_norm, w_in,
              hgrn_lower_bounds, hgrn_out_norm, w_out, ffn2_norm, ffn2_w_gate,
              ffn2_w_up, ffn2_w_down, final_norm):
    lb_all = jnp.cumsum(jax.nn.softmax(hgrn_lower_bounds.astype(jnp.float32), axis=0), axis=0)
    for layer in range(DEPTH):
        h = rmsnorm(x, ffn1_norm[layer])
        x = x + MACARON_WEIGHT * swiglu(h, ffn1_w_gate[layer], ffn1_w_up[layer], ffn1_w_down[layer])
        h = rmsnorm(x, mix_norm[layer])
        x = x + hybrid_mixer(h, w_in[layer], lb_all[layer], hgrn_out_norm[layer], w_out[layer])
        h = rmsnorm(x, ffn2_norm[layer])
        x = x + MACARON_WEIGHT * swiglu(h, ffn2_w_gate[layer], ffn2_w_up[layer], ffn2_w_down[layer])
    return rmsnorm(x, final_norm)
```

```python
import math
from contextlib import ExitStack
import numpy as np
import ml_dtypes
import concourse.bass as bass
import concourse.mybir as mybir
from concourse.bass_utils import run_bass_kernel_spmd

F32 = mybir.dt.float32
BF16 = mybir.dt.bfloat16
AF = mybir.ActivationFunctionType
ALU = mybir.AluOpType
AX = mybir.AxisListType
NEG = -30000.0
EPS = 1e-6


class Cfg:
    def __init__(self, D=4096, FF=11008, NH=16, P=2048, O=2048, groups=(29, 29, 28), debug=False):
        self.D, self.FF, self.NH, self.P, self.O = D, FF, NH, P, O
        self.DC, self.FC = D // 128, FF // 128
        self.groups = list(groups)
        assert sum(self.groups) == self.FC
        self.T = 512
        self.NTP, self.NTO = P // 512, O // 512
        self.S = P + O
        self.NB = self.S // 256
        self.NQT = O // 128
        self.NCK = self.S // 128
        self.NPJ = 7 * NH
        self.debug = debug
        self.conv_frac = 0.6
        self.conv_every = 9
        self.stagger = 3


class Res:
    __slots__ = ("name", "w", "rd", "sem", "semv")

    def __init__(self, name):
        self.name = name
        self.w = None
        self.rd = {}
        self.sem = None
        self.semv = 0


class Builder:
    def __init__(self, cfg):
        self.cfg = cfg
        self.nc = nc = bass.Bass("TRN2", target_bir_lowering=False)
        self.engs = {"pe": nc.tensor, "act": nc.scalar, "dve": nc.vector, "pool": nc.gpsimd, "sp": nc.sync}
        self.sems = {e: nc.alloc_semaphore("sem_" + e) for e in self.engs}
        self.cnt = {e: 0 for e in self.engs}
        self.waited = {e: {} for e in self.engs}
        self.dma_res = []
        self.planning = False
        self.nres = 0
        self.wcache, self.wcres = {}, {}
        self.wuse = {}
        self.multi_use = {"ffn1_g", "ffn1_u", "ffn1_d", "ffn2_g", "ffn2_u", "ffn2_d", "w_in", "w_out"}

    def res(self, name):
        return Res(name)

    def _wait(self, e, deps):
        for k, v in deps.items():
            if e == "pe" and k == "pe":
                continue
            if self.waited[e].get(k, 0) < v:
                self.engs[e].wait_ge(self.sems[k], v)
                self.waited[e][k] = v

    @staticmethod
    def _deps(reads, writes):
        deps = {}
        for r in reads:
            if r.w is not None:
                k, v = r.w
                deps[k] = max(deps.get(k, 0), v)
        for r in writes:
            if r.w is not None:
                k, v = r.w
                deps[k] = max(deps.get(k, 0), v)
            for k, v in r.rd.items():
                deps[k] = max(deps.get(k, 0), v)
        return deps

    def op(self, e, fn, reads=(), writes=()):
        if self.planning:
            return
        self._wait(e, self._deps(reads, writes))
        ins = fn(self.engs[e])
        self.cnt[e] += 1
        ins.then_inc(self.sems[e], 1)
        c = self.cnt[e]
        for r in reads:
            r.rd[e] = c
        for r in writes:
            r.w = (e, c)
            r.rd = {}

    def dma(self, q, out, in_, owner, reads=(), writes=()):
        if self.planning:
            return
        if owner.sem is None:
            key = "d%d" % len(self.dma_res)
            owner.sem = key
            self.sems[key] = self.nc.alloc_semaphore("sem_" + key)
            self.dma_res.append(owner)
        self._wait(q, self._deps(reads, writes))
        owner.semv += 16
        self.engs[q].dma_start(out=out, in_=in_).then_inc(self.sems[owner.sem], 16)
        for r in reads:
            r.rd[owner.sem] = owner.semv
        for r in writes:
            r.w = (owner.sem, owner.semv)
            r.rd = {}

    def dbg(self, name, ap, shape, dt, reads):
        if self.planning or not self.cfg.debug:
            return
        self.dbgn = getattr(self, "dbgn", set())
        if name in self.dbgn:
            return
        self.dbgn.add(name)
        d = self.nc.dram_tensor(name, list(shape), dt, kind="ExternalOutput").ap()
        r = Res(name)
        self.dma("sp", d, ap, r, reads=reads)

    def barrier(self):
        if self.planning:
            return
        deps = {e: self.cnt[e] for e in self.engs if self.cnt[e] > 0}
        for r in self.dma_res:
            deps[r.sem] = r.semv
        for e in self.engs:
            d = dict(deps)
            self.waited[e] = {k: v for k, v in self.waited[e].items()}
            for k, v in d.items():
                if self.waited[e].get(k, 0) < v:
                    self.engs[e].wait_ge(self.sems[k], v)
                    self.waited[e][k] = v

    def cache_for(self, kind, shape, key):
        wname, idx, ntiles = key
        ck = (kind, wname)
        if ck not in self.wcache:
            self.wcache[ck] = self.nc.dram_tensor("wc_%s_%s" % ck, [ntiles] + list(shape), BF16, kind="Internal").ap()
            self.wcres[ck] = {}
        return self.wcache[ck][idx], self.wcres[ck]

    def conv_jobs_c(self):
        cfg = self.cfg
        DC, FC = cfg.DC, cfg.FC
        ngmax = max(cfg.groups)
        jobs = []
        wv = self.w_out.rearrange("(c p) n -> p c n", p=128)
        for dc in range(DC):
            jobs.append(("wk", [128, DC, 128], ("w_out", dc, DC), wv[:, :, dc * 128:(dc + 1) * 128], None))
        wg, wu, wd = self.wts["ffn2"]
        wgv = wg.rearrange("(c p) n -> p c n", p=128)
        wuv = wu.rearrange("(c p) n -> p c n", p=128)
        wdv = wd.rearrange("(f p) n -> p f n", p=128)
        f0 = 0
        for gi, ng in enumerate(cfg.groups):
            for fi in range(ng):
                f = f0 + fi
                jobs.append(("wk", [128, DC, 128], ("ffn2_g", f, FC), wgv[:, :, f * 128:(f + 1) * 128], None))
                jobs.append(("wk", [128, DC, 128], ("ffn2_u", f, FC), wuv[:, :, f * 128:(f + 1) * 128], None))
            for dc in range(DC):
                jobs.append(("wd", [128, ngmax, 128], ("ffn2_d", gi * DC + dc, len(cfg.groups) * DC),
                             wdv[:, f0:f0 + ng, dc * 128:(dc + 1) * 128], ng))
            f0 += ng
        return jobs

    class WPool:
        def __init__(self, b, sb, name, n, shape):
            self.b, self.name, self.n, self.shape = b, name, n, shape
            self.tiles = [sb("%s%d" % (name, i), shape, BF16) for i in range(n)]
            self.res = [Res("%s%d" % (name, i)) for i in range(n)]
            self.plan = []
            self.issued = 0
            self.used = 0

        def _cache(self, key):
            return self.b.cache_for(self.name[1:], self.shape, key)

        def _issue(self):
            b = self.b
            j = self.issued
            shape_fn, src, key = self.plan[j]
            s = j % self.n
            cache_ap, cres = self._cache(key)
            idx = key[1]
            if idx in cres and self.name[1:] not in b.cfg.__dict__.get("no_cached_load", ""):
                b.dma("pool", self.tiles[s][:], cache_ap, self.res[s], reads=[cres[idx]], writes=[self.res[s]])
            else:
                b.dma("pool", shape_fn(self.tiles[s]), src, self.res[s], writes=[self.res[s]])
                uk = (self.name[1:], key[0], idx)
                b.wuse[uk] = b.wuse.get(uk, 0) + 1
                if key[0] in b.multi_use and b.wuse[uk] == (idx % b.cfg.stagger) + 1:
                    cres[idx] = Res("wc")
                    b.dma("sp", cache_ap, self.tiles[s][:], self.res[s], reads=[self.res[s]], writes=[cres[idx]])
            self.issued += 1

        def next(self, shape_fn, src, key):
            b = self.b
            if b.planning:
                self.plan.append((shape_fn, src, key))
                return self.tiles[0], self.res[0]
            j = self.used
            while self.issued <= j:
                self._issue()
            self.used += 1
            s = j % self.n
            return self.tiles[s], self.res[s]

        def after_use(self):
            if self.b.planning:
                return
            while self.issued < min(len(self.plan), self.used + self.n):
                self._issue()

    def build(self):
        cfg, nc = self.cfg, self.nc
        D, FF, NH, S, O, DC = cfg.D, cfg.FF, cfg.NH, cfg.S, cfg.O, cfg.DC
        W = NH * 128
        di = lambda n, shp, dt=F32: nc.dram_tensor(n, list(shp), dt, kind="ExternalInput").ap()
        self.x = di("x", [S, D])
        self.wts = {}
        for f in ("ffn1", "ffn2"):
            self.wts[f] = (di(f + "_w_gate", [D, FF]), di(f + "_w_up", [D, FF]), di(f + "_w_down", [FF, D]))
        self.w_in = di("w_in", [D, 7 * W])
        self.w_out = di("w_out", [2 * W, D])
        self.gains_d = di("gains", [4, D])
        self.hg_d = di("hgrn_out_norm", [128, 1])
        self.lbd = di("hgrn_lower_bounds", [2, W])
        self.pmask_d = di("pmask", [cfg.NQT, 128, cfg.NB])
        self.c_identf = di("c_identf", [128, 128])
        self.c_identb = di("c_identb", [128, 128], BF16)
        self.c_onesb = di("c_onesb", [128, 128], BF16)
        self.c_triu = di("c_triu", [128, 128])
        self.c_scanm = di("c_scanm", [128, 2048])
        self.c_ak = di("c_ak", [NH, 3, 128, 256])
        self.c_iota = di("c_iota", [128, cfg.NB])
        self.y = nc.dram_tensor("y", [O, D], F32, kind="ExternalOutput").ap()
        kind = "ExternalOutput" if cfg.debug else "Internal"
        ds = lambda n, shp, dt=F32: nc.dram_tensor(n, list(shp), dt, kind=kind).ap()
        self.XS = ds("XS", [cfg.NTO, 128, DC, 512])
        self.LF = ds("LF", [NH, 128, S])
        self.KF = ds("KF", [NH, 128, S])
        self.QH = ds("QH", [NH, 128, O])
        self.GH = ds("GH", [NH, 128, O])
        self.VH = ds("VH", [NH, cfg.NCK, 128, 128], BF16)
        self.MQ = ds("MQ", [NH, 128, O], BF16)
        self.MK = ds("MK", [NH, 128, S], BF16)
        self.MV = ds("MV", [NH, cfg.NCK, 128, 128], BF16)
        self.OM = ds("OM", [2 * NH, 128, O], BF16)

        sb = nc.alloc_sbuf_tensor
        self.identf = sb("identf", [128, 128], F32)
        self.identb = sb("identb", [128, 128], BF16)
        self.onesb = sb("onesb", [128, 128], BF16)
        self.gains = sb("gains_sb", [128, 4, DC], F32)
        self.lb2 = sb("lb2", [128, 2, NH], F32)
        self.lb = sb("lb", [128, NH], F32)
        self.oml = sb("oml", [128, NH], F32)
        self.hgain = sb("hgain", [128, 1], F32)
        self.r_const = Res("const")
        cr = self.r_const
        self.dma("sp", self.identf[:], self.c_identf, cr, writes=[cr])
        self.dma("sp", self.identb[:], self.c_identb, cr, writes=[cr])
        self.dma("sp", self.onesb[:], self.c_onesb, cr, writes=[cr])
        self.dma("sp", self.hgain[:], self.hg_d, cr, writes=[cr])
        with nc.allow_non_contiguous_dma(reason="tiny one-time parameter loads"):
            self.dma("sp", self.gains[:], self.gains_d.rearrange("g (c p) -> p g c", p=128), cr, writes=[cr])
            self.dma("sp", self.lb2[:], self.lbd.rearrange("r (h p) -> p r h", p=128), cr, writes=[cr])
        r_lb = Res("lb")
        self.op("dve", lambda v: v.tensor_tensor(self.lb[:], self.lb2[:, 0, :], self.lb2[:, 1, :], ALU.subtract),
                reads=[cr], writes=[r_lb])
        self.op("act", lambda a: a.activation(self.lb[:], self.lb[:], AF.Sigmoid), reads=[r_lb], writes=[r_lb])
        self.op("dve", lambda v: v.tensor_scalar(self.oml[:], self.lb[:], -1.0, 1.0, ALU.mult, ALU.add),
                reads=[r_lb], writes=[r_lb])
        self.r_lb = r_lb

        self.phase_ac(first=True)
        self.phase_b()
        self.phase_ac(first=False)
        return nc

    def phase_ac(self, first):
        cfg, nc = self.cfg, self.nc
        DC = cfg.DC
        ngmax = max(cfg.groups)
        es = ExitStack()
        sb = lambda *a: es.enter_context(nc.sbuf_tensor(*a))
        ps = lambda *a: es.enter_context(nc.psum_tensor(*a))
        tag = "A" if first else "C"
        T = {}
        T["xT"] = sb(tag + "xT", [128, DC, 512], F32)
        T["hT"] = sb(tag + "hT", [128, DC, 512], BF16)
        T["aT"] = sb(tag + "aT", [128, ngmax, 512], BF16)
        T["xin"] = [sb(tag + "xin%d" % i, [128, 512], F32) for i in range(2)]
        T["sq"] = [sb(tag + "sq%d" % i, [128, 512], BF16) for i in range(2)]
        T["rstd"] = sb(tag + "rstd", [128, 512], F32)
        T["sg"] = [sb(tag + "sg%d" % i, [128, 512], F32) for i in range(2)]
        T["stf"] = [sb(tag + "stf%d" % i, [128, 512], F32) for i in range(4)]
        T["stb"] = [sb(tag + "stb%d" % i, [128, 512], BF16) for i in range(3)]
        T["stv"] = [sb(tag + "stv%d" % i, [128, 4, 128], BF16) for i in range(2)]
        T["psA"] = [ps(tag + "psA%d" % i, [128, 512], F32) for i in range(4)]
        T["psT"] = [ps(tag + "psT%d" % i, [128, 4, 128], F32) for i in range(2)]
        T["psTb"] = ps(tag + "psTb", [128, 4, 128], BF16)
        T["psS"] = ps(tag + "psS", [128, 512], F32)
        R = {k: ([Res(k + str(i)) for i in range(len(v))] if isinstance(v, list) else Res(k)) for k, v in T.items()}
        R["xTc"] = [Res("xTc%d" % c) for c in range(DC)]
        R["hTc"] = [Res("hTc%d" % c) for c in range(DC)]
        R["aTc"] = [Res("aTc%d" % c) for c in range(ngmax)]
        self.T, self.R = T, R
        self.rr = {k: 0 for k in T}
        wk = Builder.WPool(self, sb, tag + "wk", 4, [128, DC, 128])
        wdk = Builder.WPool(self, sb, tag + "wd", 2, [128, ngmax, 128])
        self.wk, self.wdk = wk, wdk

        def body():
            if first:
                for tt in range(cfg.NTP + cfg.NTO):
                    own = tt >= cfg.NTP
                    self.load_x(tt)
                    self.dbg("DBG_x", T["xT"][:], [128, DC, 512], F32, R["xTc"])
                    self.norm(0)
                    self.dbg("DBG_h", T["hT"][:], [128, DC, 512], BF16, R["hTc"])
                    self.dbg("DBG_rstd", T["rstd"][:], [128, 512], F32, [R["rstd"]])
                    self.ffn("ffn1")
                    self.norm(1)
                    self.proj_in(tt, own)
                    if own:
                        k = tt - cfg.NTP
                        self.dma("sp", self.XS[k], T["xT"][:], R["xT"], reads=R["xTc"])
            else:
                for k in range(cfg.NTO):
                    self.dma("sp", T["xT"][:], self.XS[k], R["xT"], writes=R["xTc"])
                    with nc.allow_non_contiguous_dma(reason="1KB rows"):
                        self.dma("sp", T["hT"][:], self.OM.rearrange("c p t -> p c t")[:, :, k * 512:(k + 1) * 512],
                                 R["hT"], writes=R["hTc"])
                    self.proj_out()
                    self.norm(2)
                    self.ffn("ffn2")
                    self.final(k)

        self.planning = True
        body()
        self.planning = False
        self.rr = {k: 0 for k in T}
        body()
        assert wk.used == len(wk.plan) and wdk.used == len(wdk.plan), (wk.used, len(wk.plan), wdk.used, len(wdk.plan))
        self.barrier()
        es.close()

    def rot(self, k):
        i = self.rr[k]
        self.rr[k] = (i + 1) % len(self.T[k])
        return self.T[k][i], self.R[k][i]

    def load_x(self, tt):
        cfg, T, R = self.cfg, self.T, self.R
        DC = cfg.DC
        G = min(4, DC)
        for sub in range(4):
            r0 = tt * 512 + sub * 128
            for cg in range(DC // G):
                xin, rx = self.rot("xin")
                self.dma("sp", xin[:, 0:G * 128], self.x[r0:r0 + 128, cg * G * 128:(cg + 1) * G * 128], rx, writes=[rx])
                pt, rp = self.rot("psT")
                for k in range(G):
                    self.op("pe", lambda p, k=k: p.transpose(pt[:, k, :], xin[:, k * 128:(k + 1) * 128], self.identf[:]),
                            reads=[rx, self.r_const], writes=[rp])
                dst = T["xT"][:, cg * G:(cg + 1) * G, sub * 128:(sub + 1) * 128]
                self.op("act", lambda a: a.copy(dst, pt[:, 0:G, :]), reads=[rp],
                        writes=[R["xTc"][c] for c in range(cg * G, (cg + 1) * G)])

    def norm_stats(self):
        cfg, T, R = self.cfg, self.T, self.R
        DC = cfg.DC
        for c in range(DC):
            sq, rs = self.rot("sq")
            self.op("act", lambda a: a.activation(sq[:], T["xT"][:, c, :], AF.Square), reads=[R["xTc"][c]], writes=[rs])
            self.op("pe", lambda p: p.matmul(T["psS"][:], self.onesb[:], sq[:], start=(c == 0), stop=(c == DC - 1)),
                    reads=[rs, self.r_const], writes=[R["psS"]])
        self.op("act", lambda a: a.activation(T["rstd"][:], T["psS"][:], AF.Sqrt, bias=EPS, scale=1.0 / cfg.D),
                reads=[R["psS"]], writes=[R["rstd"]])
        self.op("dve", lambda v: v.reciprocal(T["rstd"][:], T["rstd"][:]), reads=[R["rstd"]], writes=[R["rstd"]])

    def norm(self, gi):
        cfg, T, R = self.cfg, self.T, self.R
        self.norm_stats()
        for c in range(cfg.DC):
            self.op("dve", lambda v: v.scalar_tensor_tensor(T["hT"][:, c, :], T["xT"][:, c, :], self.gains[:, gi, c:c + 1],
                                                             T["rstd"][:], ALU.mult, ALU.mult),
                    reads=[R["xTc"][c], R["rstd"], self.r_const], writes=[R["hTc"][c]])

    def ffn(self, name):
        cfg, T, R = self.cfg, self.T, self.R
        DC = cfg.DC
        wg, wu, wd = self.wts[name]
        wgv = wg.rearrange("(c p) n -> p c n", p=128)
        wuv = wu.rearrange("(c p) n -> p c n", p=128)
        wdv = wd.rearrange("(f p) n -> p f n", p=128)
        f0 = 0
        for gi, ng in enumerate(cfg.groups):
            for fi in range(ng):
                f = f0 + fi
                tg, rg = self.wk.next(lambda t: t[:], wgv[:, :, f * 128:(f + 1) * 128], (name + "_g", f, cfg.FC))
                tu, ru = self.wk.next(lambda t: t[:], wuv[:, :, f * 128:(f + 1) * 128], (name + "_u", f, cfg.FC))
                pg, rpg = self.rot("psA")
                pu, rpu = self.rot("psA")
                for c in range(DC):
                    self.op("pe", lambda p: p.matmul(pg[:], tg[:, c, :], T["hT"][:, c, :], start=(c == 0), stop=(c == DC - 1)),
                            reads=[rg, R["hTc"][c]], writes=[rpg])
                for c in range(DC):
                    self.op("pe", lambda p: p.matmul(pu[:], tu[:, c, :], T["hT"][:, c, :], start=(c == 0), stop=(c == DC - 1)),
                            reads=[ru, R["hTc"][c]], writes=[rpu])
                self.wk.after_use()
                sg, rsg = self.rot("sg")
                self.op("act", lambda a: a.activation(sg[:], pg[:], AF.Silu), reads=[rpg], writes=[rsg])
                self.dbg("DBG_sg", sg[:], [128, 512], F32, [rsg])
                self.dbg("DBG_wg", tg[:], [128, DC, 128], BF16, [rg])
                self.op("dve", lambda v: v.tensor_tensor(T["aT"][:, fi, :], sg[:], pu[:], ALU.mult),
                        reads=[rsg, rpu], writes=[R["aTc"][fi]])
            self.dbg("DBG_a", T["aT"][:], [128, max(cfg.groups), 512], BF16, R["aTc"])
            for dc in range(DC):
                td, rd = self.wdk.next(lambda t, ng=ng: t[:, 0:ng, :], wdv[:, f0:f0 + ng, dc * 128:(dc + 1) * 128], (name + "_d", gi * DC + dc, len(cfg.groups) * DC))
                py, rpy = self.rot("psA")
                for fi in range(ng):
                    self.op("pe", lambda p: p.matmul(py[:], td[:, fi, :], T["aT"][:, fi, :], start=(fi == 0), stop=(fi == ng - 1)),
                            reads=[rd, R["aTc"][fi]], writes=[rpy])
                self.wdk.after_use()
                self.op("dve", lambda v: v.scalar_tensor_tensor(T["xT"][:, dc, :], py[:], 0.5, T["xT"][:, dc, :], ALU.mult, ALU.add),
                        reads=[rpy], writes=[R["xTc"][dc]])
            f0 += ng

    def proj_out(self):
        cfg, T, R = self.cfg, self.T, self.R
        DC = cfg.DC
        wv = self.w_out.rearrange("(c p) n -> p c n", p=128)
        for dc in range(DC):
            tw, rw = self.wk.next(lambda t: t[:], wv[:, :, dc * 128:(dc + 1) * 128], ("w_out", dc, DC))
            py, rpy = self.rot("psA")
            for c in range(DC):
                self.op("pe", lambda p: p.matmul(py[:], tw[:, c, :], T["hT"][:, c, :], start=(c == 0), stop=(c == DC - 1)),
                        reads=[rw, R["hTc"][c]], writes=[rpy])
            self.wk.after_use()
            self.op("dve", lambda v: v.scalar_tensor_tensor(T["xT"][:, dc, :], py[:], 1.0, T["xT"][:, dc, :], ALU.mult, ALU.add),
                    reads=[rpy], writes=[R["xTc"][dc]])

    def proj_in(self, tt, own):
        cfg, T, R = self.cfg, self.T, self.R
        DC, NH = cfg.DC, cfg.NH
        wv = self.w_in.rearrange("(c p) n -> p c n", p=128)
        t0 = tt * 512
        o0 = t0 - cfg.P
        for j in range(cfg.NPJ):
            kind, h = j // NH, j % NH
            if not own and kind in (0, 3, 4):
                continue
            tw, rw = self.wk.next(lambda t: t[:], wv[:, :, j * 128:(j + 1) * 128], ("w_in", j, cfg.NPJ))
            pp, rpp = self.rot("psA")
            for c in range(DC):
                self.op("pe", lambda p: p.matmul(pp[:], tw[:, c, :], T["hT"][:, c, :], start=(c == 0), stop=(c == DC - 1)),
                        reads=[rw, R["hTc"][c]], writes=[rpp])
            self.wk.after_use()
            if kind in (0, 3):
                st, rs = self.rot("stf")
                self.op("act", lambda a: a.activation(st[:], pp[:], AF.Silu), reads=[rpp], writes=[rs])
                dst = (self.QH if kind == 0 else self.GH)[h, :, o0:o0 + 512]
                self.dma("sp", dst, st[:], rs, reads=[rs])
            elif kind == 1:
                s1, r1 = self.rot("stf")
                s2, r2 = self.rot("stf")
                self.op("act", lambda a: a.activation(s1[:], pp[:], AF.Sigmoid), reads=[rpp], writes=[r1])
                self.op("dve", lambda v: v.tensor_scalar(s1[:], s1[:], self.oml[:, h:h + 1], self.lb[:, h:h + 1], ALU.mult, ALU.add),
                        reads=[r1, self.r_lb], writes=[r1])
                self.op("dve", lambda v: v.tensor_scalar(s2[:], s1[:], -1.0, 1.0, ALU.mult, ALU.add), reads=[r1], writes=[r2])
                self.op("act", lambda a: a.activation(s1[:], s1[:], AF.Ln), reads=[r1], writes=[r1])
                self.dma("sp", self.LF[h, :, t0:t0 + 512], s1[:], r1, reads=[r1])
                self.dma("sp", self.KF[h, :, t0:t0 + 512], s2[:], r2, reads=[r2])
            elif kind in (4, 5):
                st, rs = self.rot("stb")
                self.op("act", lambda a: a.copy(st[:], pp[:]), reads=[rpp], writes=[rs])
                dst = self.MQ[h, :, o0:o0 + 512] if kind == 4 else self.MK[h, :, t0:t0 + 512]
                self.dma("sp", dst, st[:], rs, reads=[rs])
            else:
                st, rs = self.rot("stb")
                self.op("act", lambda a: a.copy(st[:], pp[:]), reads=[rpp], writes=[rs])
                for sub in range(4):
                    self.op("pe", lambda p: p.transpose(T["psTb"][:, sub, :], st[:, sub * 128:(sub + 1) * 128], self.identb[:]),
                            reads=[rs, self.r_const], writes=[R["psTb"]])
                sv, rv = self.rot("stv")
                self.op("dve", lambda v: v.tensor_copy(sv[:], T["psTb"][:]), reads=[R["psTb"]], writes=[rv])
                dstT = (self.VH if kind == 2 else self.MV)[h, tt * 4:(tt + 1) * 4].rearrange("s p d -> p s d")
                with self.nc.allow_non_contiguous_dma(reason="256B rows"):
                    self.dma("sp", dstT, sv[:], rv, reads=[rv])

    def final(self, k):
        cfg, T, R = self.cfg, self.T, self.R
        DC = cfg.DC
        G = min(4, DC)
        self.norm_stats()
        for cg in range(DC // G):
            tiles = []
            for i in range(G):
                c = cg * G + i
                st, rs = self.rot("stf")
                self.op("dve", lambda v: v.scalar_tensor_tensor(st[:], T["xT"][:, c, :], self.gains[:, 3, c:c + 1],
                                                                 T["rstd"][:], ALU.mult, ALU.mult),
                        reads=[R["xTc"][c], R["rstd"], self.r_const], writes=[rs])
                tiles.append((st, rs))
            for sub in range(4):
                pt, rp = self.rot("psT")
                for i, (st, rs) in enumerate(tiles):
                    self.op("pe", lambda p: p.transpose(pt[:, i, :], st[:, sub * 128:(sub + 1) * 128], self.identf[:]),
                            reads=[rs, self.r_const], writes=[rp])
                xo, ro = self.rot("xin")
                self.op("act", lambda a: a.copy(xo[:, 0:G * 128], pt[:, 0:G, :].rearrange("p a b -> p (a b)")), reads=[rp], writes=[ro])
                r0 = k * 512 + sub * 128
                self.dma("sp", self.y[r0:r0 + 128, cg * G * 128:(cg + 1) * G * 128], xo[:, 0:G * 128], ro, reads=[ro])

    def phase_b(self):
        self.phase_b_hgrn()
        self.phase_b_moba()

    def phase_b_hgrn(self):
        cfg, nc = self.cfg, self.nc
        NH, S, O, P, NCK = cfg.NH, cfg.S, cfg.O, cfg.P, cfg.NCK
        es = ExitStack()
        sb = lambda *a: es.enter_context(nc.sbuf_tensor(*a))
        ps = lambda *a: es.enter_context(nc.psum_tensor(*a))
        op, dma = self.op, self.dma
        cr = self.r_const
        triu = sb("triu", [128, 128], F32)
        scanm = sb("scanm", [128, 2048], F32)
        rB = Res("constB")
        dma("sp", triu[:], self.c_triu, rB, writes=[rB])
        dma("sp", scanm[:], self.c_scanm, rB, writes=[rB])
        pb = [ps("pb%d" % i, [128, 512], F32) for i in range(6)]
        rpb = [Res("pb%d" % i) for i in range(6)]
        ptb = [ps("ptb%d" % i, [128, 4, 128], BF16) for i in range(2)]
        rptb = [Res("ptb%d" % i) for i in range(2)]
        st = {"pb": 0, "ptb": 0}

        def rot_pb():
            i = st["pb"]; st["pb"] = (i + 1) % 6
            return pb[i], rpb[i]

        def rot_ptb():
            i = st["ptb"]; st["ptb"] = (i + 1) % 2
            return ptb[i], rptb[i]

        OC = O // 128
        PC = P // 128
        inb = []
        for i in range(2):
            d = dict(lf=sb("lf%d" % i, [128, S], F32), kf=sb("kf%d" % i, [128, S], F32), qs=sb("qs%d" % i, [128, O], F32),
                     gg=sb("gg%d" % i, [128, O], F32), vtok=sb("vtok%d" % i, [128, NCK, 128], BF16))
            d["r"] = {k: Res(k + str(i)) for k in ("lf", "kf", "qs", "gg", "vtok")}
            inb.append(d)
        e1 = sb("e1", [128, 2048], F32)
        khat = sb("khat", [128, S], BF16); qhat = sb("qhat", [128, O], BF16)
        ktok = sb("ktok", [128, NCK, 128], BF16)
        kvs = sb("kvs", [128, NCK, 128], F32)
        scmall = sb("scmall", [128, OC, 128], BF16); sball = sb("sball", [128, OC, 128], BF16)
        oall = sb("oall", [128, O], F32); sqo = sb("sqo", [128, O], BF16); ofin = sb("ofin", [128, O], BF16)
        rst = sb("rst", [128, O], F32)
        mm = sb("mm", [128, NCK], F32); ll = sb("ll", [128, NCK], F32)
        em = sb("em", [128, NCK], F32); el = sb("el", [128, NCK], F32); elm = sb("elm", [128, NCK], F32)
        Sf = sb("Sf", [128, 128], F32)
        names = ["e1", "khat", "qhat", "ktok", "oall", "sqo", "ofin", "rst", "mm", "ll", "em", "el", "elm", "Sf"]
        r = {n: Res(n) for n in names}
        rkv = [Res("kvs%d" % c) for c in range(NCK)]
        rscm = [Res("scm%d" % c) for c in range(OC)]
        rsb = [Res("sb%d" % c) for c in range(OC)]

        def loads(h):
            b = inb[h % 2]
            dma("sp", b["lf"][:], self.LF[h], b["r"]["lf"], writes=[b["r"]["lf"]])
            dma("sp", b["kf"][:], self.KF[h], b["r"]["kf"], writes=[b["r"]["kf"]])
            dma("sp", b["qs"][:], self.QH[h], b["r"]["qs"], writes=[b["r"]["qs"]])
            dma("sp", b["gg"][:], self.GH[h], b["r"]["gg"], writes=[b["r"]["gg"]])
            with nc.allow_non_contiguous_dma(reason="256B rows"):
                dma("sp", b["vtok"][:], self.VH[h].rearrange("c p d -> p c d"), b["r"]["vtok"], writes=[b["r"]["vtok"]])

        loads(0)
        for h in range(NH):
            if h + 1 < NH:
                loads(h + 1)
            b = inb[h % 2]
            lf, kf, qs, gg, vtok = b["lf"], b["kf"], b["qs"], b["gg"], b["vtok"]
            rl, rk, rq, rg, rv = (b["r"][k] for k in ("lf", "kf", "qs", "gg", "vtok"))
            lf3 = lf[:].rearrange("p (c t) -> p c t", t=128)
            for s0 in range(0, S, 2048):
                n = min(2048, S - s0)
                op("dve", lambda v: v.tensor_tensor_scan(lf[:, s0:s0 + n], scanm[:, 0:n], lf[:, s0:s0 + n], 0.0, ALU.mult, ALU.add),
                   reads=[rB], writes=[rl])
            op("dve", lambda v: v.tensor_copy(mm[:], lf3[:, :, 63]), reads=[rl], writes=[r["mm"]])
            op("dve", lambda v: v.tensor_copy(ll[:], lf3[:, :, 127]), reads=[rl], writes=[r["ll"]])
            op("act", lambda a: a.activation(em[:], mm[:], AF.Exp), reads=[r["mm"]], writes=[r["em"]])
            op("act", lambda a: a.activation(el[:], ll[:], AF.Exp), reads=[r["ll"]], writes=[r["el"]])
            op("dve", lambda v: v.tensor_tensor(elm[:], ll[:], mm[:], ALU.subtract), reads=[r["ll"], r["mm"]], writes=[r["elm"]])
            op("act", lambda a: a.activation(elm[:], elm[:], AF.Exp), reads=[r["elm"]], writes=[r["elm"]])
            op("dve", lambda v: v.tensor_tensor(lf3, lf3, mm[:].unsqueeze(2).to_broadcast([128, NCK, 128]), ALU.subtract),
               reads=[r["mm"]], writes=[rl])
            for s0 in range(0, S, 2048):
                n = min(2048, S - s0)
                op("act", lambda a: a.activation(e1[:, 0:n], lf[:, s0:s0 + n], AF.Exp, scale=-1.0), reads=[rl], writes=[r["e1"]])
                op("pool", lambda g: g.tensor_tensor(khat[:, s0:s0 + n], kf[:, s0:s0 + n], e1[:, 0:n], ALU.mult),
                   reads=[rk, r["e1"]], writes=[r["khat"]])
            for s0 in range(0, O, 2048):
                n = min(2048, O - s0)
                op("act", lambda a: a.activation(e1[:, 0:n], lf[:, P + s0:P + s0 + n], AF.Exp), reads=[rl], writes=[r["e1"]])
                op("dve", lambda v: v.scalar_tensor_tensor(qhat[:, s0:s0 + n], qs[:, s0:s0 + n], 128.0 ** -0.5, e1[:, 0:n], ALU.mult, ALU.mult),
                   reads=[rq, r["e1"]], writes=[r["qhat"]])
            for c0 in range(0, NCK, 4):
                pt, rp = rot_ptb()
                for i in range(4):
                    c = c0 + i
                    op("pe", lambda p: p.transpose(pt[:, i, :], khat[:, c * 128:(c + 1) * 128], self.identb[:]),
                       reads=[r["khat"], cr], writes=[rp])
                op("act", lambda a: a.copy(ktok[:, c0:c0 + 4, :], pt[:]), reads=[rp], writes=[r["ktok"]])
            for c in range(NCK - 1):
                pk, rpk = rot_pb()
                op("pe", lambda p: p.matmul(pk[:, 0:128], ktok[:, c, :], vtok[:, c, :], start=True, stop=True),
                   reads=[r["ktok"], rv], writes=[rpk])
                op("act", lambda a: a.activation(kvs[:, c, :], pk[:, 0:128], AF.Copy, scale=elm[:, c:c + 1]),
                   reads=[rpk, r["elm"]], writes=[rkv[c]])
            for co in range(OC):
                c = PC + co
                psc, rsc = rot_pb()
                op("pe", lambda p: p.matmul(psc[:, 0:128], khat[:, c * 128:(c + 1) * 128], qhat[:, co * 128:(co + 1) * 128], start=True, stop=True),
                   reads=[r["khat"], r["qhat"]], writes=[rsc])
                op("dve", lambda v: v.tensor_tensor(scmall[:, co, :], psc[:, 0:128], triu[:], ALU.mult), reads=[rsc, rB], writes=[rscm[co]])
            op("dve", lambda v: v.memset(Sf[:], 0.0), writes=[r["Sf"]])
            for c in range(NCK):
                co = c - PC
                if co >= 0:
                    op("dve", lambda v: v.tensor_scalar(sball[:, co, :], Sf[:], em[:, c:c + 1], None, ALU.mult),
                       reads=[r["Sf"], r["em"]], writes=[rsb[co]])
                if c < NCK - 1:
                    op("dve", lambda v: v.scalar_tensor_tensor(Sf[:], Sf[:], el[:, c:c + 1], kvs[:, c, :], ALU.mult, ALU.add),
                       reads=[rkv[c], r["el"]], writes=[r["Sf"]])
            for co in range(OC):
                c = PC + co
                po, rpo = rot_pb()
                op("pe", lambda p: p.matmul(po[:, 0:128], vtok[:, c, :], scmall[:, co, :], start=True, stop=False),
                   reads=[rv, rscm[co]], writes=[rpo])
                op("pe", lambda p: p.matmul(po[:, 0:128], sball[:, co, :], qhat[:, co * 128:(co + 1) * 128], start=False, stop=True),
                   reads=[rsb[co], r["qhat"]], writes=[rpo])
                op("act", lambda a: a.copy(oall[:, co * 128:(co + 1) * 128], po[:, 0:128]), reads=[rpo], writes=[r["oall"]])
            op("act", lambda a: a.activation(sqo[:], oall[:], AF.Square), reads=[r["oall"]], writes=[r["sqo"]])
            for s0 in range(0, O, 512):
                pss, rss = rot_pb()
                op("pe", lambda p: p.matmul(pss[:], self.onesb[:], sqo[:, s0:s0 + 512], start=True, stop=True),
                   reads=[r["sqo"], cr], writes=[rss])
                op("act", lambda a: a.activation(rst[:, s0:s0 + 512], pss[:], AF.Sqrt, bias=EPS, scale=1.0 / 128), reads=[rss], writes=[r["rst"]])
            op("dve", lambda v: v.reciprocal(rst[:], rst[:]), reads=[r["rst"]], writes=[r["rst"]])
            op("dve", lambda v: v.scalar_tensor_tensor(oall[:], oall[:], self.hgain[:, 0:1], rst[:], ALU.mult, ALU.mult),
               reads=[r["rst"], cr], writes=[r["oall"]])
            op("pool", lambda g: g.tensor_tensor(ofin[:], oall[:], gg[:], ALU.mult), reads=[r["oall"], rg], writes=[r["ofin"]])
            dma("sp", self.OM[h], ofin[:], r["ofin"], reads=[r["ofin"]])
        self.barrier()
        es.close()

    def phase_b_moba(self):
        cfg, nc = self.cfg, self.nc
        NH, S, O, P, NCK, NB, NQT = cfg.NH, cfg.S, cfg.O, cfg.P, cfg.NCK, cfg.NB, cfg.NQT
        es = ExitStack()
        sb = lambda *a: es.enter_context(nc.sbuf_tensor(*a))
        ps = lambda *a: es.enter_context(nc.psum_tensor(*a))
        op, dma = self.op, self.dma
        cr = self.r_const
        pmask = sb("pmask_sb", [128, NQT, NB], F32)
        iota = sb("iota", [128, NB], F32)
        rB = Res("constM")
        dma("sp", iota[:], self.c_iota, rB, writes=[rB])
        with nc.allow_non_contiguous_dma(reason="tiny"):
            dma("sp", pmask[:], self.pmask_d.rearrange("q p n -> p q n"), rB, writes=[rB])
        NSTR = 2 if NH >= 2 else 1
        pb = [ps("mpb%d" % i, [128, 512], F32) for i in range(2)]
        rpb = [Res("mpb%d" % i) for i in range(2)]
        ptb = [ps("mptb%d" % i, [128, 4, 128], BF16) for i in range(2)]
        rptb = [Res("mptb%d" % i) for i in range(2)]
        st = {"pb": 0, "ptb": 0}

        def rot_pb():
            i = st["pb"]; st["pb"] = (i + 1) % 2
            return pb[i], rpb[i]

        def rot_ptb():
            i = st["ptb"]; st["ptb"] = (i + 1) % 2
            return ptb[i], rptb[i]

        scale = 128.0 ** -0.5
        NS = 3

        def make_stream(si):
            z = "s%d" % si
            d = {}
            d["pacc"] = [ps("pacc%s_%d" % (z, i), [128, 512], F32) for i in range(2)]
            d["rpacc"] = [Res("pacc") for i in range(2)]
            d["inb"] = []
            for i in range(2):
                e = dict(qT=sb("qT%s_%d" % (z, i), [128, O], BF16), kT=sb("kT%s_%d" % (z, i), [128, S], BF16),
                         mv=sb("mv%s_%d" % (z, i), [128, NCK, 128], BF16), ak3=sb("ak3%s_%d" % (z, i), [128, 3, 256], F32))
                e["r"] = {k: Res(k) for k in ("qT", "kT", "mv", "ak3")}
                d["inb"].append(e)
            for n_, shp, dt in [("km", [128, NB], F32), ("kmb", [128, NB], BF16), ("gm", [128, NB], F32), ("top8", [128, 8], F32),
                                ("b1", [128, NB], F32), ("rb", [128, NQT, NB], F32), ("tq", [128, NQT, NB], F32),
                                ("rsum", [128, NQT, NB + 1], F32), ("lsum", [128, 1], F32), ("on", [128, 128], BF16)]:
                d[n_] = sb(n_ + z, shp, dt)
            d["ssb"] = [sb("ssb%s_%d" % (z, i), [128, 256], F32) for i in range(NS)]
            d["pbf"] = [sb("pbf%s_%d" % (z, i), [128, 256], BF16) for i in range(NS)]
            d["pT"] = [sb("pT%s_%d" % (z, i), [128, 2, 128], BF16) for i in range(NS)]
            d["rssb"] = [Res("ssb") for i in range(NS)]
            d["rpbf"] = [Res("pbf") for i in range(NS)]
            d["rpT"] = [Res("pT") for i in range(NS)]
            d["rrs"] = [Res("rsum") for q in range(NQT)]
            d["oT"] = [sb("oT%s_%d" % (z, i), [128, O], BF16) for i in range(2)]
            d["roT"] = [Res("oT") for i in range(2)]
            d["r"] = {n_: Res(n_) for n_ in ["km", "kmb", "gm", "top8", "b1", "lsum", "on"]}
            d["rrb"] = [Res("rb") for q in range(NQT)]
            d["rtq"] = [Res("tq") for q in range(NQT)]
            return d

        def loads(d, h, slot):
            b = d["inb"][slot]
            dma("sp", b["qT"][:], self.MQ[h], b["r"]["qT"], writes=[b["r"]["qT"]])
            dma("sp", b["kT"][:], self.MK[h], b["r"]["kT"], writes=[b["r"]["kT"]])
            with nc.allow_non_contiguous_dma(reason="256B rows"):
                dma("sp", b["mv"][:], self.MV[h].rearrange("c p d -> p c d"), b["r"]["mv"], writes=[b["r"]["mv"]])
            dma("sp", b["ak3"][:], self.c_ak[h].rearrange("a p j -> p a j"), b["r"]["ak3"], writes=[b["r"]["ak3"]])

        def stream_gen(d, heads):
            r = d["r"]
            km, kmb, gm, top8, b1, rb, tq, rsum, lsum, on = (d[k] for k in ("km", "kmb", "gm", "top8", "b1", "rb", "tq", "rsum", "lsum", "on"))
            ssb, pbf, pT, rssb, rpbf, rpT, rrs, rrb, rtq = (d[k] for k in ("ssb", "pbf", "pT", "rssb", "rpbf", "rpT", "rrs", "rrb", "rtq"))
            loads(d, heads[0], 0)
            for hi, h in enumerate(heads):
                if hi + 1 < len(heads):
                    loads(d, heads[hi + 1], (hi + 1) % 2)
                b = d["inb"][hi % 2]
                qT, kT, mv, ak3 = b["qT"], b["kT"], b["mv"], b["ak3"]
                rq, rk, rv, rak = (b["r"][k] for k in ("qT", "kT", "mv", "ak3"))
                oTh, roTh = d["oT"][hi % 2], d["roT"][hi % 2]
                slope = float(np.exp2(np.float32(-8.0) * np.float32(h + 1) / np.float32(NH)))
                op("dve", lambda v: v.tensor_reduce(km[:], kT[:].rearrange("p (n l) -> p n l", l=256), AX.X, ALU.add),
                   reads=[rk], writes=[r["km"]])
                op("dve", lambda v: v.tensor_scalar(kmb[:], km[:], 1.0 / 256, None, ALU.mult), reads=[r["km"]], writes=[r["kmb"]])
                yield
                info = []
                for qt in range(NQT):
                    qq = qT[:, qt * 128:(qt + 1) * 128]
                    tpos = P + qt * 128
                    ob = tpos // 256
                    half = (tpos % 256) // 128
                    info.append((qq, ob, half))
                    pg, rpg = rot_pb()
                    op("pe", lambda p: p.matmul(pg[:, 0:NB], qq, kmb[:], start=True, stop=True), reads=[rq, r["kmb"]], writes=[rpg])
                    op("dve", lambda v: v.tensor_tensor(gm[:], pg[:, 0:NB], pmask[:, qt, :], ALU.add), reads=[rpg, rB], writes=[r["gm"]])
                    op("dve", lambda v: v.max(top8[:], gm[:]), reads=[r["gm"]], writes=[r["top8"]])
                    op("dve", lambda v: v.tensor_scalar(b1[:], gm[:], top8[:, 2:3], None, ALU.is_ge), reads=[r["gm"], r["top8"]], writes=[r["b1"]])
                    op("dve", lambda v: v.scalar_tensor_tensor(b1[:], gm[:], NEG / 2, b1[:], ALU.is_gt, ALU.mult), reads=[r["gm"]], writes=[r["b1"]])
                    op("dve", lambda v: v.tensor_scalar(tq[:, qt, :], iota[:], float(tpos), -slope, ALU.add, ALU.mult), reads=[rB], writes=[rtq[qt]])
                    op("dve", lambda v: v.tensor_scalar(rb[:, qt, :], b1[:], -1.0, -NEG, ALU.add, ALU.mult), reads=[r["b1"]], writes=[rrb[qt]])
                    op("dve", lambda v: v.tensor_tensor(rb[:, qt, :], rb[:, qt, :], tq[:, qt, :], ALU.add), reads=[rtq[qt]], writes=[rrb[qt]])
                    yield
                pairs = [(qt, n) for qt in range(NQT) for n in range(info[qt][1] + 1)]

                def stA(k):
                    qt, n = pairs[k]
                    qq, ob, half = info[qt]
                    i3 = k % NS
                    pss, rss = rot_pb()
                    op("pe", lambda p: p.matmul(pss[:, 0:256], qq, kT[:, n * 256:(n + 1) * 256], start=True, stop=True),
                       reads=[rq, rk], writes=[rss])
                    a = 0 if n < ob else 1 + half
                    bias = rb[:, qt, n:n + 1] if n < ob else tq[:, qt, n:n + 1]
                    rbias = rrb[qt] if n < ob else rtq[qt]
                    op("dve", lambda v: v.tensor_tensor(ssb[i3][:], pss[:, 0:256], ak3[:, a, :], ALU.add), reads=[rss, rak], writes=[rssb[i3]])
                    op("act", lambda a_: a_.activation(pbf[i3][:], ssb[i3][:], AF.Exp, bias=bias, scale=scale, accum_out=rsum[:, qt, n:n + 1]),
                       reads=[rssb[i3], rbias], writes=[rpbf[i3], rrs[qt]])

                def stB(k):
                    i3 = k % NS
                    pt, rp = rot_ptb()
                    for hf in range(2):
                        op("pe", lambda p: p.transpose(pt[:, hf, :], pbf[i3][:, hf * 128:(hf + 1) * 128], self.identb[:]),
                           reads=[rpbf[i3], cr], writes=[rp])
                    if k % 2:
                        op("dve", lambda v: v.tensor_copy(pT[i3][:], pt[:, 0:2, :]), reads=[rp], writes=[rpT[i3]])
                    else:
                        op("act", lambda a_: a_.copy(pT[i3][:], pt[:, 0:2, :]), reads=[rp], writes=[rpT[i3]])

                def stC(k):
                    qt, n = pairs[k]
                    qq, ob, half = info[qt]
                    i3 = k % NS
                    po, rpo = d["pacc"][qt % 2], d["rpacc"][qt % 2]
                    for hf in range(2):
                        op("pe", lambda p: p.matmul(po[:, 0:128], pT[i3][:, hf, :], mv[:, 2 * n + hf, :],
                                                    start=(n == 0 and hf == 0), stop=(n == ob and hf == 1)),
                           reads=[rpT[i3], rv], writes=[rpo])
                    if n == ob:
                        op("dve", lambda v: v.tensor_reduce(lsum[:], rsum[:, qt, 0:ob + 1], AX.X, ALU.add), reads=[rrs[qt]], writes=[r["lsum"]])
                        op("dve", lambda v: v.reciprocal(lsum[:], lsum[:]), reads=[r["lsum"]], writes=[r["lsum"]])
                        op("dve", lambda v: v.tensor_scalar(on[:], po[:, 0:128], lsum[:, 0:1], None, ALU.mult), reads=[rpo, r["lsum"]], writes=[r["on"]])
                        pt, rp = rot_ptb()
                        op("pe", lambda p: p.transpose(pt[:, 0, :], on[:], self.identb[:]), reads=[r["on"], cr], writes=[rp])
                        op("act", lambda a_: a_.copy(oTh[:, qt * 128:(qt + 1) * 128], pt[:, 0, :]), reads=[rp], writes=[roTh])

                for step in range(len(pairs) + 2):
                    if step < len(pairs):
                        stA(step)
                    if 0 <= step - 1 < len(pairs):
                        stB(step - 1)
                    if 0 <= step - 2 < len(pairs):
                        stC(step - 2)
                    yield
                dma("sp", self.OM[NH + h], oTh[:], roTh, reads=[roTh])

        jobs = self.conv_jobs_c()
        jobs = jobs[:int(len(jobs) * cfg.conv_frac)]
        ngmax = max(cfg.groups)
        cst = {"wk": [sb("cwk%d" % i, [128, cfg.DC, 128], BF16) for i in range(3)],
               "wd": [sb("cwd%d" % i, [128, ngmax, 128], BF16) for i in range(2)]}
        rcst = {k: [Res("cst") for _ in v] for k, v in cst.items()}
        cnt = {"wk": 0, "wd": 0}

        def conv_step():
            if not jobs:
                return
            kind, shape, key, src_ap, ng = jobs.pop(0)
            cache_ap, cres = self.cache_for(kind, shape, key)
            if key[1] in cres:
                return
            i = cnt[kind] % len(cst[kind]); cnt[kind] += 1
            t, rt = cst[kind][i], rcst[kind][i]
            dst = t[:] if ng is None else t[:, 0:ng, :]
            dma("pool", dst, src_ap, rt, writes=[rt])
            cres[key[1]] = Res("wc")
            dma("sp", cache_ap, t[:], rt, reads=[rt], writes=[cres[key[1]]])

        gens = [stream_gen(make_stream(si), list(range(si, NH, NSTR))) for si in range(NSTR)]
        active = list(gens)
        it = 0
        while active:
            for g in list(active):
                try:
                    next(g)
                except StopIteration:
                    active.remove(g)
            it += 1
            if it % cfg.conv_every == 0:
                conv_step()
        self.barrier()
        es.close()


def make_consts(cfg):
    NH = cfg.NH
    c = {}
    c["c_identf"] = np.eye(128, dtype=np.float32)
    c["c_identb"] = np.eye(128, dtype=np.float32).astype(ml_dtypes.bfloat16)
    c["c_onesb"] = np.ones((128, 128), np.float32).astype(ml_dtypes.bfloat16)
    c["c_triu"] = np.triu(np.ones((128, 128), np.float32))
    sm = np.ones((128, 2048), np.float32)
    sm[:, ::128] = 0.0
    c["c_scanm"] = sm
    scale = np.float32(128.0 ** -0.5)
    j = np.arange(256, dtype=np.float32)[None, :]
    i = np.arange(128, dtype=np.float32)[:, None]
    ak = np.zeros((NH, 3, 128, 256), np.float32)
    for h in range(NH):
        slope = np.exp2(np.float32(-8.0) * np.float32(h + 1) / np.float32(NH)).astype(np.float32)
        base = np.broadcast_to(slope * j / scale, (128, 256))
        ak[h, 0] = base
        for half in range(2):
            ak[h, 1 + half] = np.where(j <= i + 128 * half, base, np.float32(NEG) / scale)
    c["c_ak"] = ak
    c["c_iota"] = (i - 256.0 * np.arange(cfg.NB, dtype=np.float32)[None, :]).astype(np.float32)
    return c


def make_pmask(cfg, has_prefix):
    pm = np.full((cfg.NQT, 128, cfg.NB), NEG, np.float32)
    first = 0 if has_prefix else cfg.P // 256
    for qt in range(cfg.NQT):
        ob = (cfg.P + qt * 128) // 256
        pm[qt, :, first:ob] = 0.0
    return pm


_CACHE = {}


def run(cfg, inputs, n_batch, trace=False):
    if "nc" not in _CACHE or _CACHE.get("key") != (cfg.D, cfg.FF, cfg.NH, cfg.P, cfg.O, cfg.debug):
        _CACHE["nc"] = Builder(cfg).build()
        _CACHE["key"] = (cfg.D, cfg.FF, cfg.NH, cfg.P, cfg.O, cfg.debug)
    nc = _CACHE["nc"]
    f32 = lambda a: np.ascontiguousarray(np.asarray(a, dtype=np.float32))
    x = f32(inputs["x"])
    shared = dict(make_consts(cfg))
    for f in ("ffn1", "ffn2"):
        for w in ("w_gate", "w_up", "w_down"):
            shared[f + "_" + w] = f32(inputs[f + "_" + w][0])
    shared["w_in"] = f32(inputs["w_in"][0])
    shared["w_out"] = f32(inputs["w_out"][0])
    shared["gains"] = np.stack([f32(inputs["ffn1_norm"][0]), f32(inputs["mix_norm"][0]),
                                f32(inputs["ffn2_norm"][0]), f32(inputs["final_norm"])])
    shared["hgrn_out_norm"] = f32(inputs["hgrn_out_norm"][0]).reshape(128, 1)
    shared["hgrn_lower_bounds"] = f32(inputs["hgrn_lower_bounds"])
    pm = [make_pmask(cfg, False), make_pmask(cfg, True)]
    in_maps = []
    for c in range(2 * n_batch):
        b, s = c // 2, c % 2
        xc = np.zeros((cfg.S, cfg.D), np.float32)
        if s == 0:
            xc[cfg.P:] = x[b, :cfg.O]
        else:
            xc[:] = x[b]
        m = dict(shared)
        m["x"] = xc
        m["pmask"] = pm[s]
        in_maps.append(m)
    res = run_bass_kernel_spmd(nc, in_maps, core_ids=list(range(2 * n_batch)), trace=trace)
    out = np.zeros((n_batch, 2 * cfg.O, cfg.D), np.float32)
    for c in range(2 * n_batch):
        b, s = c // 2, c % 2
        out[b, s * cfg.O:(s + 1) * cfg.O] = res.results[c]["y"]
    return out, res


def kernel(**inputs):
    cfg = Cfg()
    out, _ = run(cfg, inputs, 4)
    return out
```

```python
import math
from contextlib import ExitStack
import numpy as np
import ml_dtypes
import concourse.bass as bass
import concourse.mybir as mybir
from concourse.bass_utils import run_bass_kernel_spmd

F32 = mybir.dt.float32
BF16 = mybir.dt.bfloat16
AF = mybir.ActivationFunctionType
ALU = mybir.AluOpType
AX = mybir.AxisListType
NEG = -30000.0
EPS = 1e-6


class Cfg:
    def __init__(self, D=4096, FF=11008, NH=16, P=2048, O=2048, groups=(29, 29, 28), debug=False):
        self.D, self.FF, self.NH, self.P, self.O = D, FF, NH, P, O
        self.DC, self.FC = D // 128, FF // 128
        self.groups = list(groups)
        assert sum(self.groups) == self.FC
        self.T = 512
        self.NTP, self.NTO = P // 512, O // 512
        self.S = P + O
        self.NB = self.S // 256
        self.NQT = O // 128
        self.NCK = self.S // 128
        self.NPJ = 7 * NH
        self.debug = debug
        self.conv_frac = 0.6
        self.conv_every = 9


class Res:
    __slots__ = ("name", "w", "rd", "sem", "semv")

    def __init__(self, name):
        self.name = name
        self.w = None
        self.rd = {}
        self.sem = None
        self.semv = 0


class Builder:
    def __init__(self, cfg):
        self.cfg = cfg
        self.nc = nc = bass.Bass("TRN2", target_bir_lowering=False)
        self.engs = {"pe": nc.tensor, "act": nc.scalar, "dve": nc.vector, "pool": nc.gpsimd, "sp": nc.sync}
        self.sems = {e: nc.alloc_semaphore("sem_" + e) for e in self.engs}
        self.cnt = {e: 0 for e in self.engs}
        self.waited = {e: {} for e in self.engs}
        self.dma_res = []
        self.planning = False
        self.nres = 0
        self.wcache, self.wcres = {}, {}
        self.multi_use = {"ffn1_g", "ffn1_u", "ffn1_d", "ffn2_g", "ffn2_u", "ffn2_d", "w_in", "w_out"}

    def res(self, name):
        return Res(name)

    def _wait(self, e, deps):
        for k, v in deps.items():
            if e == "pe" and k == "pe":
                continue
            if self.waited[e].get(k, 0) < v:
                self.engs[e].wait_ge(self.sems[k], v)
                self.waited[e][k] = v

    @staticmethod
    def _deps(reads, writes):
        deps = {}
        for r in reads:
            if r.w is not None:
                k, v = r.w
                deps[k] = max(deps.get(k, 0), v)
        for r in writes:
            if r.w is not None:
                k, v = r.w
                deps[k] = max(deps.get(k, 0), v)
            for k, v in r.rd.items():
                deps[k] = max(deps.get(k, 0), v)
        return deps

    def op(self, e, fn, reads=(), writes=()):
        if self.planning:
            return
        self._wait(e, self._deps(reads, writes))
        ins = fn(self.engs[e])
        self.cnt[e] += 1
        ins.then_inc(self.sems[e], 1)
        c = self.cnt[e]
        for r in reads:
            r.rd[e] = c
        for r in writes:
            r.w = (e, c)
            r.rd = {}

    def dma(self, q, out, in_, owner, reads=(), writes=()):
        if self.planning:
            return
        if owner.sem is None:
            key = "d%d" % len(self.dma_res)
            owner.sem = key
            self.sems[key] = self.nc.alloc_semaphore("sem_" + key)
            self.dma_res.append(owner)
        self._wait(q, self._deps(reads, writes))
        owner.semv += 16
        self.engs[q].dma_start(out=out, in_=in_).then_inc(self.sems[owner.sem], 16)
        for r in reads:
            r.rd[owner.sem] = owner.semv
        for r in writes:
            r.w = (owner.sem, owner.semv)
            r.rd = {}

    def dbg(self, name, ap, shape, dt, reads):
        if self.planning or not self.cfg.debug:
            return
        self.dbgn = getattr(self, "dbgn", set())
        if name in self.dbgn:
            return
        self.dbgn.add(name)
        d = self.nc.dram_tensor(name, list(shape), dt, kind="ExternalOutput").ap()
        r = Res(name)
        self.dma("sp", d, ap, r, reads=reads)

    def barrier(self):
        if self.planning:
            return
        deps = {e: self.cnt[e] for e in self.engs if self.cnt[e] > 0}
        for r in self.dma_res:
            deps[r.sem] = r.semv
        for e in self.engs:
            d = dict(deps)
            self.waited[e] = {k: v for k, v in self.waited[e].items()}
            for k, v in d.items():
                if self.waited[e].get(k, 0) < v:
                    self.engs[e].wait_ge(self.sems[k], v)
                    self.waited[e][k] = v

    def cache_for(self, kind, shape, key):
        wname, idx, ntiles = key
        ck = (kind, wname)
        if ck not in self.wcache:
            self.wcache[ck] = self.nc.dram_tensor("wc_%s_%s" % ck, [ntiles] + list(shape), BF16, kind="Internal").ap()
            self.wcres[ck] = {}
        return self.wcache[ck][idx], self.wcres[ck]

    def conv_jobs_c(self):
        cfg = self.cfg
        DC, FC = cfg.DC, cfg.FC
        ngmax = max(cfg.groups)
        jobs = []
        wv = self.w_out.rearrange("(c p) n -> p c n", p=128)
        for dc in range(DC):
            jobs.append(("wk", [128, DC, 128], ("w_out", dc, DC), wv[:, :, dc * 128:(dc + 1) * 128], None))
        wg, wu, wd = self.wts["ffn2"]
        wgv = wg.rearrange("(c p) n -> p c n", p=128)
        wuv = wu.rearrange("(c p) n -> p c n", p=128)
        wdv = wd.rearrange("(f p) n -> p f n", p=128)
        f0 = 0
        for gi, ng in enumerate(cfg.groups):
            for fi in range(ng):
                f = f0 + fi
                jobs.append(("wk", [128, DC, 128], ("ffn2_g", f, FC), wgv[:, :, f * 128:(f + 1) * 128], None))
                jobs.append(("wk", [128, DC, 128], ("ffn2_u", f, FC), wuv[:, :, f * 128:(f + 1) * 128], None))
            for dc in range(DC):
                jobs.append(("wd", [128, ngmax, 128], ("ffn2_d", gi * DC + dc, len(cfg.groups) * DC),
                             wdv[:, f0:f0 + ng, dc * 128:(dc + 1) * 128], ng))
            f0 += ng
        return jobs

    class WPool:
        def __init__(self, b, sb, name, n, shape):
            self.b, self.name, self.n, self.shape = b, name, n, shape
            self.tiles = [sb("%s%d" % (name, i), shape, BF16) for i in range(n)]
            self.res = [Res("%s%d" % (name, i)) for i in range(n)]
            self.plan = []
            self.issued = 0
            self.used = 0

        def _cache(self, key):
            return self.b.cache_for(self.name[1:], self.shape, key)

        def _issue(self):
            b = self.b
            j = self.issued
            shape_fn, src, key = self.plan[j]
            s = j % self.n
            cache_ap, cres = self._cache(key)
            idx = key[1]
            if idx in cres and self.name[1:] not in b.cfg.__dict__.get("no_cached_load", ""):
                b.dma("pool", self.tiles[s][:], cache_ap, self.res[s], reads=[cres[idx]], writes=[self.res[s]])
            else:
                b.dma("pool", shape_fn(self.tiles[s]), src, self.res[s], writes=[self.res[s]])
                if key[0] in b.multi_use:
                    cres[idx] = Res("wc")
                    b.dma("sp", cache_ap, self.tiles[s][:], self.res[s], reads=[self.res[s]], writes=[cres[idx]])
            self.issued += 1

        def next(self, shape_fn, src, key):
            b = self.b
            if b.planning:
                self.plan.append((shape_fn, src, key))
                return self.tiles[0], self.res[0]
            j = self.used
            while self.issued <= j:
                self._issue()
            self.used += 1
            s = j % self.n
            return self.tiles[s], self.res[s]

        def after_use(self):
            if self.b.planning:
                return
            while self.issued < min(len(self.plan), self.used + self.n):
                self._issue()

    def build(self):
        cfg, nc = self.cfg, self.nc
        D, FF, NH, S, O, DC = cfg.D, cfg.FF, cfg.NH, cfg.S, cfg.O, cfg.DC
        W = NH * 128
        di = lambda n, shp, dt=F32: nc.dram_tensor(n, list(shp), dt, kind="ExternalInput").ap()
        self.x = di("x", [S, D])
        self.wts = {}
        for f in ("ffn1", "ffn2"):
            self.wts[f] = (di(f + "_w_gate", [D, FF]), di(f + "_w_up", [D, FF]), di(f + "_w_down", [FF, D]))
        self.w_in = di("w_in", [D, 7 * W])
        self.w_out = di("w_out", [2 * W, D])
        self.gains_d = di("gains", [4, D])
        self.hg_d = di("hgrn_out_norm", [128, 1])
        self.lbd = di("hgrn_lower_bounds", [2, W])
        self.pmask_d = di("pmask", [cfg.NQT, 128, cfg.NB])
        self.c_identf = di("c_identf", [128, 128])
        self.c_identb = di("c_identb", [128, 128], BF16)
        self.c_onesb = di("c_onesb", [128, 128], BF16)
        self.c_triu = di("c_triu", [128, 128])
        self.c_scanm = di("c_scanm", [128, 2048])
        self.c_akr = di("c_akr", [NH, 128, 256], BF16)
        self.c_akc = di("c_akc", [NH, 2, 2, 128, 256], BF16)
        self.c_iota = di("c_iota", [128, cfg.NB])
        self.y = nc.dram_tensor("y", [O, D], F32, kind="ExternalOutput").ap()
        kind = "ExternalOutput" if cfg.debug else "Internal"
        ds = lambda n, shp, dt=F32: nc.dram_tensor(n, list(shp), dt, kind=kind).ap()
        self.XS = ds("XS", [cfg.NTO, 128, DC, 512])
        self.LF = ds("LF", [NH, 128, S])
        self.KF = ds("KF", [NH, 128, S])
        self.QH = ds("QH", [NH, 128, O])
        self.GH = ds("GH", [NH, 128, O])
        self.VH = ds("VH", [NH, cfg.NCK, 128, 128], BF16)
        self.MQ = ds("MQ", [NH, 128, O], BF16)
        self.MK = ds("MK", [NH, 128, S], BF16)
        self.MV = ds("MV", [NH, cfg.NCK, 128, 128], BF16)
        self.OM = ds("OM", [2 * NH, 128, O], BF16)

        sb = nc.alloc_sbuf_tensor
        self.identf = sb("identf", [128, 128], F32)
        self.identb = sb("identb", [128, 128], BF16)
        self.onesb = sb("onesb", [128, 128], BF16)
        self.gains = sb("gains_sb", [128, 4, DC], F32)
        self.lb2 = sb("lb2", [128, 2, NH], F32)
        self.lb = sb("lb", [128, NH], F32)
        self.oml = sb("oml", [128, NH], F32)
        self.hgain = sb("hgain", [128, 1], F32)
        self.r_const = Res("const")
        cr = self.r_const
        self.dma("sp", self.identf[:], self.c_identf, cr, writes=[cr])
        self.dma("sp", self.identb[:], self.c_identb, cr, writes=[cr])
        self.dma("sp", self.onesb[:], self.c_onesb, cr, writes=[cr])
        self.dma("sp", self.hgain[:], self.hg_d, cr, writes=[cr])
        with nc.allow_non_contiguous_dma(reason="tiny one-time parameter loads"):
            self.dma("sp", self.gains[:], self.gains_d.rearrange("g (c p) -> p g c", p=128), cr, writes=[cr])
            self.dma("sp", self.lb2[:], self.lbd.rearrange("r (h p) -> p r h", p=128), cr, writes=[cr])
        r_lb = Res("lb")
        self.op("dve", lambda v: v.tensor_tensor(self.lb[:], self.lb2[:, 0, :], self.lb2[:, 1, :], ALU.subtract),
                reads=[cr], writes=[r_lb])
        self.op("act", lambda a: a.activation(self.lb[:], self.lb[:], AF.Sigmoid), reads=[r_lb], writes=[r_lb])
        self.op("dve", lambda v: v.tensor_scalar(self.oml[:], self.lb[:], -1.0, 1.0, ALU.mult, ALU.add),
                reads=[r_lb], writes=[r_lb])
        self.r_lb = r_lb

        self.phase_ac(first=True)
        self.phase_b()
        self.phase_ac(first=False)
        return nc

    def phase_ac(self, first):
        cfg, nc = self.cfg, self.nc
        DC = cfg.DC
        ngmax = max(cfg.groups)
        es = ExitStack()
        sb = lambda *a: es.enter_context(nc.sbuf_tensor(*a))
        ps = lambda *a: es.enter_context(nc.psum_tensor(*a))
        tag = "A" if first else "C"
        T = {}
        T["xT"] = sb(tag + "xT", [128, DC, 512], F32)
        T["hT"] = sb(tag + "hT", [128, DC, 512], BF16)
        T["aT"] = sb(tag + "aT", [128, ngmax, 512], BF16)
        T["xin"] = [sb(tag + "xin%d" % i, [128, 512], F32) for i in range(2)]
        T["sq"] = [sb(tag + "sq%d" % i, [128, 512], BF16) for i in range(2)]
        T["rstd"] = sb(tag + "rstd", [128, 512], F32)
        T["sg"] = [sb(tag + "sg%d" % i, [128, 512], F32) for i in range(2)]
        T["stf"] = [sb(tag + "stf%d" % i, [128, 512], F32) for i in range(4)]
        T["stb"] = [sb(tag + "stb%d" % i, [128, 512], BF16) for i in range(3)]
        T["stv"] = [sb(tag + "stv%d" % i, [128, 4, 128], BF16) for i in range(2)]
        T["psA"] = [ps(tag + "psA%d" % i, [128, 512], F32) for i in range(4)]
        T["psT"] = [ps(tag + "psT%d" % i, [128, 4, 128], F32) for i in range(2)]
        T["psTb"] = ps(tag + "psTb", [128, 4, 128], BF16)
        T["psS"] = ps(tag + "psS", [128, 512], F32)
        R = {k: ([Res(k + str(i)) for i in range(len(v))] if isinstance(v, list) else Res(k)) for k, v in T.items()}
        R["xTc"] = [Res("xTc%d" % c) for c in range(DC)]
        R["hTc"] = [Res("hTc%d" % c) for c in range(DC)]
        R["aTc"] = [Res("aTc%d" % c) for c in range(ngmax)]
        self.T, self.R = T, R
        self.rr = {k: 0 for k in T}
        wk = Builder.WPool(self, sb, tag + "wk", 4, [128, DC, 128])
        wdk = Builder.WPool(self, sb, tag + "wd", 2, [128, ngmax, 128])
        self.wk, self.wdk = wk, wdk

        def body():
            if first:
                for tt in range(cfg.NTP + cfg.NTO):
                    own = tt >= cfg.NTP
                    self.load_x(tt)
                    self.dbg("DBG_x", T["xT"][:], [128, DC, 512], F32, R["xTc"])
                    self.norm(0)
                    self.dbg("DBG_h", T["hT"][:], [128, DC, 512], BF16, R["hTc"])
                    self.dbg("DBG_rstd", T["rstd"][:], [128, 512], F32, [R["rstd"]])
                    self.ffn("ffn1")
                    self.norm(1)
                    self.proj_in(tt, own)
                    if own:
                        k = tt - cfg.NTP
                        self.dma("sp", self.XS[k], T["xT"][:], R["xT"], reads=R["xTc"])
            else:
                for k in range(cfg.NTO):
                    self.dma("sp", T["xT"][:], self.XS[k], R["xT"], writes=R["xTc"])
                    with nc.allow_non_contiguous_dma(reason="1KB rows"):
                        self.dma("sp", T["hT"][:], self.OM.rearrange("c p t -> p c t")[:, :, k * 512:(k + 1) * 512],
                                 R["hT"], writes=R["hTc"])
                    self.proj_out()
                    self.norm(2)
                    self.ffn("ffn2")
                    self.final(k)

        self.planning = True
        body()
        self.planning = False
        self.rr = {k: 0 for k in T}
        body()
        assert wk.used == len(wk.plan) and wdk.used == len(wdk.plan), (wk.used, len(wk.plan), wdk.used, len(wdk.plan))
        self.barrier()
        es.close()

    def rot(self, k):
        i = self.rr[k]
        self.rr[k] = (i + 1) % len(self.T[k])
        return self.T[k][i], self.R[k][i]

    def load_x(self, tt):
        cfg, T, R = self.cfg, self.T, self.R
        DC = cfg.DC
        G = min(4, DC)
        for sub in range(4):
            r0 = tt * 512 + sub * 128
            for cg in range(DC // G):
                xin, rx = self.rot("xin")
                self.dma("sp", xin[:, 0:G * 128], self.x[r0:r0 + 128, cg * G * 128:(cg + 1) * G * 128], rx, writes=[rx])
                pt, rp = self.rot("psT")
                for k in range(G):
                    self.op("pe", lambda p, k=k: p.transpose(pt[:, k, :], xin[:, k * 128:(k + 1) * 128], self.identf[:]),
                            reads=[rx, self.r_const], writes=[rp])
                dst = T["xT"][:, cg * G:(cg + 1) * G, sub * 128:(sub + 1) * 128]
                self.op("act", lambda a: a.copy(dst, pt[:, 0:G, :]), reads=[rp],
                        writes=[R["xTc"][c] for c in range(cg * G, (cg + 1) * G)])

    def norm_stats(self):
        cfg, T, R = self.cfg, self.T, self.R
        DC = cfg.DC
        for c in range(DC):
            sq, rs = self.rot("sq")
            self.op("act", lambda a: a.activation(sq[:], T["xT"][:, c, :], AF.Square), reads=[R["xTc"][c]], writes=[rs])
            self.op("pe", lambda p: p.matmul(T["psS"][:], self.onesb[:], sq[:], start=(c == 0), stop=(c == DC - 1)),
                    reads=[rs, self.r_const], writes=[R["psS"]])
        self.op("act", lambda a: a.activation(T["rstd"][:], T["psS"][:], AF.Sqrt, bias=EPS, scale=1.0 / cfg.D),
                reads=[R["psS"]], writes=[R["rstd"]])
        self.op("dve", lambda v: v.reciprocal(T["rstd"][:], T["rstd"][:]), reads=[R["rstd"]], writes=[R["rstd"]])

    def norm(self, gi):
        cfg, T, R = self.cfg, self.T, self.R
        self.norm_stats()
        for c in range(cfg.DC):
            self.op("dve", lambda v: v.scalar_tensor_tensor(T["hT"][:, c, :], T["xT"][:, c, :], self.gains[:, gi, c:c + 1],
                                                             T["rstd"][:], ALU.mult, ALU.mult),
                    reads=[R["xTc"][c], R["rstd"], self.r_const], writes=[R["hTc"][c]])

    def ffn(self, name):
        cfg, T, R = self.cfg, self.T, self.R
        DC = cfg.DC
        wg, wu, wd = self.wts[name]
        wgv = wg.rearrange("(c p) n -> p c n", p=128)
        wuv = wu.rearrange("(c p) n -> p c n", p=128)
        wdv = wd.rearrange("(f p) n -> p f n", p=128)
        f0 = 0
        for gi, ng in enumerate(cfg.groups):
            for fi in range(ng):
                f = f0 + fi
                tg, rg = self.wk.next(lambda t: t[:], wgv[:, :, f * 128:(f + 1) * 128], (name + "_g", f, cfg.FC))
                tu, ru = self.wk.next(lambda t: t[:], wuv[:, :, f * 128:(f + 1) * 128], (name + "_u", f, cfg.FC))
                pg, rpg = self.rot("psA")
                pu, rpu = self.rot("psA")
                for c in range(DC):
                    self.op("pe", lambda p: p.matmul(pg[:], tg[:, c, :], T["hT"][:, c, :], start=(c == 0), stop=(c == DC - 1)),
                            reads=[rg, R["hTc"][c]], writes=[rpg])
                for c in range(DC):
                    self.op("pe", lambda p: p.matmul(pu[:], tu[:, c, :], T["hT"][:, c, :], start=(c == 0), stop=(c == DC - 1)),
                            reads=[ru, R["hTc"][c]], writes=[rpu])
                self.wk.after_use()
                sg, rsg = self.rot("sg")
                self.op("act", lambda a: a.activation(sg[:], pg[:], AF.Silu), reads=[rpg], writes=[rsg])
                self.dbg("DBG_sg", sg[:], [128, 512], F32, [rsg])
                self.dbg("DBG_wg", tg[:], [128, DC, 128], BF16, [rg])
                self.op("dve", lambda v: v.tensor_tensor(T["aT"][:, fi, :], sg[:], pu[:], ALU.mult),
                        reads=[rsg, rpu], writes=[R["aTc"][fi]])
            self.dbg("DBG_a", T["aT"][:], [128, max(cfg.groups), 512], BF16, R["aTc"])
            for dc in range(DC):
                td, rd = self.wdk.next(lambda t, ng=ng: t[:, 0:ng, :], wdv[:, f0:f0 + ng, dc * 128:(dc + 1) * 128], (name + "_d", gi * DC + dc, len(cfg.groups) * DC))
                py, rpy = self.rot("psA")
                for fi in range(ng):
                    self.op("pe", lambda p: p.matmul(py[:], td[:, fi, :], T["aT"][:, fi, :], start=(fi == 0), stop=(fi == ng - 1)),
                            reads=[rd, R["aTc"][fi]], writes=[rpy])
                self.wdk.after_use()
                self.op("dve", lambda v: v.scalar_tensor_tensor(T["xT"][:, dc, :], py[:], 0.5, T["xT"][:, dc, :], ALU.mult, ALU.add),
                        reads=[rpy], writes=[R["xTc"][dc]])
            f0 += ng

    def proj_out(self):
        cfg, T, R = self.cfg, self.T, self.R
        DC = cfg.DC
        wv = self.w_out.rearrange("(c p) n -> p c n", p=128)
        for dc in range(DC):
            tw, rw = self.wk.next(lambda t: t[:], wv[:, :, dc * 128:(dc + 1) * 128], ("w_out", dc, DC))
            py, rpy = self.rot("psA")
            for c in range(DC):
                self.op("pe", lambda p: p.matmul(py[:], tw[:, c, :], T["hT"][:, c, :], start=(c == 0), stop=(c == DC - 1)),
                        reads=[rw, R["hTc"][c]], writes=[rpy])
            self.wk.after_use()
            self.op("dve", lambda v: v.scalar_tensor_tensor(T["xT"][:, dc, :], py[:], 1.0, T["xT"][:, dc, :], ALU.mult, ALU.add),
                    reads=[rpy], writes=[R["xTc"][dc]])

    def proj_in(self, tt, own):
        cfg, T, R = self.cfg, self.T, self.R
        DC, NH = cfg.DC, cfg.NH
        wv = self.w_in.rearrange("(c p) n -> p c n", p=128)
        t0 = tt * 512
        o0 = t0 - cfg.P
        for j in range(cfg.NPJ):
            kind, h = j // NH, j % NH
            if not own and kind in (0, 3, 4):
                continue
            tw, rw = self.wk.next(lambda t: t[:], wv[:, :, j * 128:(j + 1) * 128], ("w_in", j, cfg.NPJ))
            pp, rpp = self.rot("psA")
            for c in range(DC):
                self.op("pe", lambda p: p.matmul(pp[:], tw[:, c, :], T["hT"][:, c, :], start=(c == 0), stop=(c == DC - 1)),
                        reads=[rw, R["hTc"][c]], writes=[rpp])
            self.wk.after_use()
            if kind in (0, 3):
                st, rs = self.rot("stf")
                self.op("act", lambda a: a.activation(st[:], pp[:], AF.Silu), reads=[rpp], writes=[rs])
                dst = (self.QH if kind == 0 else self.GH)[h, :, o0:o0 + 512]
                self.dma("sp", dst, st[:], rs, reads=[rs])
            elif kind == 1:
                s1, r1 = self.rot("stf")
                s2, r2 = self.rot("stf")
                self.op("act", lambda a: a.activation(s1[:], pp[:], AF.Sigmoid), reads=[rpp], writes=[r1])
                self.op("dve", lambda v: v.tensor_scalar(s1[:], s1[:], self.oml[:, h:h + 1], self.lb[:, h:h + 1], ALU.mult, ALU.add),
                        reads=[r1, self.r_lb], writes=[r1])
                self.op("dve", lambda v: v.tensor_scalar(s2[:], s1[:], -1.0, 1.0, ALU.mult, ALU.add), reads=[r1], writes=[r2])
                self.op("act", lambda a: a.activation(s1[:], s1[:], AF.Ln), reads=[r1], writes=[r1])
                self.dma("sp", self.LF[h, :, t0:t0 + 512], s1[:], r1, reads=[r1])
                self.dma("sp", self.KF[h, :, t0:t0 + 512], s2[:], r2, reads=[r2])
            elif kind in (4, 5):
                st, rs = self.rot("stb")
                self.op("act", lambda a: a.copy(st[:], pp[:]), reads=[rpp], writes=[rs])
                dst = self.MQ[h, :, o0:o0 + 512] if kind == 4 else self.MK[h, :, t0:t0 + 512]
                self.dma("sp", dst, st[:], rs, reads=[rs])
            else:
                st, rs = self.rot("stb")
                self.op("act", lambda a: a.copy(st[:], pp[:]), reads=[rpp], writes=[rs])
                for sub in range(4):
                    self.op("pe", lambda p: p.transpose(T["psTb"][:, sub, :], st[:, sub * 128:(sub + 1) * 128], self.identb[:]),
                            reads=[rs, self.r_const], writes=[R["psTb"]])
                sv, rv = self.rot("stv")
                self.op("dve", lambda v: v.tensor_copy(sv[:], T["psTb"][:]), reads=[R["psTb"]], writes=[rv])
                dstT = (self.VH if kind == 2 else self.MV)[h, tt * 4:(tt + 1) * 4].rearrange("s p d -> p s d")
                with self.nc.allow_non_contiguous_dma(reason="256B rows"):
                    self.dma("sp", dstT, sv[:], rv, reads=[rv])

    def final(self, k):
        cfg, T, R = self.cfg, self.T, self.R
        DC = cfg.DC
        G = min(4, DC)
        self.norm_stats()
        for cg in range(DC // G):
            tiles = []
            for i in range(G):
                c = cg * G + i
                st, rs = self.rot("stf")
                self.op("dve", lambda v: v.scalar_tensor_tensor(st[:], T["xT"][:, c, :], self.gains[:, 3, c:c + 1],
                                                                 T["rstd"][:], ALU.mult, ALU.mult),
                        reads=[R["xTc"][c], R["rstd"], self.r_const], writes=[rs])
                tiles.append((st, rs))
            for sub in range(4):
                pt, rp = self.rot("psT")
                for i, (st, rs) in enumerate(tiles):
                    self.op("pe", lambda p: p.transpose(pt[:, i, :], st[:, sub * 128:(sub + 1) * 128], self.identf[:]),
                            reads=[rs, self.r_const], writes=[rp])
                xo, ro = self.rot("xin")
                self.op("act", lambda a: a.copy(xo[:, 0:G * 128], pt[:, 0:G, :].rearrange("p a b -> p (a b)")), reads=[rp], writes=[ro])
                r0 = k * 512 + sub * 128
                self.dma("sp", self.y[r0:r0 + 128, cg * G * 128:(cg + 1) * G * 128], xo[:, 0:G * 128], ro, reads=[ro])

    def phase_b(self):
        self.phase_b_hgrn()
        self.phase_b_moba()

    def phase_b_hgrn(self):
        cfg, nc = self.cfg, self.nc
        NH, S, O, P, NCK = cfg.NH, cfg.S, cfg.O, cfg.P, cfg.NCK
        es = ExitStack()
        sb = lambda *a: es.enter_context(nc.sbuf_tensor(*a))
        ps = lambda *a: es.enter_context(nc.psum_tensor(*a))
        op, dma = self.op, self.dma
        cr = self.r_const
        triu = sb("triu", [128, 128], F32)
        scanm = sb("scanm", [128, 2048], F32)
        rB = Res("constB")
        dma("sp", triu[:], self.c_triu, rB, writes=[rB])
        dma("sp", scanm[:], self.c_scanm, rB, writes=[rB])
        pb = [ps("pb%d" % i, [128, 512], F32) for i in range(6)]
        rpb = [Res("pb%d" % i) for i in range(6)]
        ptb = [ps("ptb%d" % i, [128, 4, 128], BF16) for i in range(2)]
        rptb = [Res("ptb%d" % i) for i in range(2)]
        st = {"pb": 0, "ptb": 0}

        def rot_pb():
            i = st["pb"]; st["pb"] = (i + 1) % 6
            return pb[i], rpb[i]

        def rot_ptb():
            i = st["ptb"]; st["ptb"] = (i + 1) % 2
            return ptb[i], rptb[i]

        OC = O // 128
        PC = P // 128
        inb = []
        for i in range(2):
            d = dict(lf=sb("lf%d" % i, [128, S], F32), kf=sb("kf%d" % i, [128, S], F32), qs=sb("qs%d" % i, [128, O], F32),
                     gg=sb("gg%d" % i, [128, O], F32), vtok=sb("vtok%d" % i, [128, NCK, 128], BF16))
            d["r"] = {k: Res(k + str(i)) for k in ("lf", "kf", "qs", "gg", "vtok")}
            inb.append(d)
        e1 = sb("e1", [128, 2048], F32)
        khat = sb("khat", [128, S], BF16); qhat = sb("qhat", [128, O], BF16)
        ktok = sb("ktok", [128, NCK, 128], BF16)
        kvs = sb("kvs", [128, NCK, 128], F32)
        scmall = sb("scmall", [128, OC, 128], BF16); sball = sb("sball", [128, OC, 128], BF16)
        oall = sb("oall", [128, O], F32); sqo = sb("sqo", [128, O], BF16); ofin = sb("ofin", [128, O], BF16)
        rst = sb("rst", [128, O], F32)
        mm = sb("mm", [128, NCK], F32); ll = sb("ll", [128, NCK], F32)
        em = sb("em", [128, NCK], F32); el = sb("el", [128, NCK], F32); elm = sb("elm", [128, NCK], F32)
        Sf = sb("Sf", [128, 128], F32)
        names = ["e1", "khat", "qhat", "ktok", "oall", "sqo", "ofin", "rst", "mm", "ll", "em", "el", "elm", "Sf"]
        r = {n: Res(n) for n in names}
        rkv = [Res("kvs%d" % c) for c in range(NCK)]
        rscm = [Res("scm%d" % c) for c in range(OC)]
        rsb = [Res("sb%d" % c) for c in range(OC)]

        def loads(h):
            b = inb[h % 2]
            dma("sp", b["lf"][:], self.LF[h], b["r"]["lf"], writes=[b["r"]["lf"]])
            dma("sp", b["kf"][:], self.KF[h], b["r"]["kf"], writes=[b["r"]["kf"]])
            dma("sp", b["qs"][:], self.QH[h], b["r"]["qs"], writes=[b["r"]["qs"]])
            dma("sp", b["gg"][:], self.GH[h], b["r"]["gg"], writes=[b["r"]["gg"]])
            with nc.allow_non_contiguous_dma(reason="256B rows"):
                dma("sp", b["vtok"][:], self.VH[h].rearrange("c p d -> p c d"), b["r"]["vtok"], writes=[b["r"]["vtok"]])

        loads(0)
        for h in range(NH):
            if h + 1 < NH:
                loads(h + 1)
            b = inb[h % 2]
            lf, kf, qs, gg, vtok = b["lf"], b["kf"], b["qs"], b["gg"], b["vtok"]
            rl, rk, rq, rg, rv = (b["r"][k] for k in ("lf", "kf", "qs", "gg", "vtok"))
            lf3 = lf[:].rearrange("p (c t) -> p c t", t=128)
            for s0 in range(0, S, 2048):
                n = min(2048, S - s0)
                op("dve", lambda v: v.tensor_tensor_scan(lf[:, s0:s0 + n], scanm[:, 0:n], lf[:, s0:s0 + n], 0.0, ALU.mult, ALU.add),
                   reads=[rB], writes=[rl])
            op("dve", lambda v: v.tensor_copy(mm[:], lf3[:, :, 63]), reads=[rl], writes=[r["mm"]])
            op("dve", lambda v: v.tensor_copy(ll[:], lf3[:, :, 127]), reads=[rl], writes=[r["ll"]])
            op("act", lambda a: a.activation(em[:], mm[:], AF.Exp), reads=[r["mm"]], writes=[r["em"]])
            op("act", lambda a: a.activation(el[:], ll[:], AF.Exp), reads=[r["ll"]], writes=[r["el"]])
            op("dve", lambda v: v.tensor_tensor(elm[:], ll[:], mm[:], ALU.subtract), reads=[r["ll"], r["mm"]], writes=[r["elm"]])
            op("act", lambda a: a.activation(elm[:], elm[:], AF.Exp), reads=[r["elm"]], writes=[r["elm"]])
            op("dve", lambda v: v.tensor_tensor(lf3, lf3, mm[:].unsqueeze(2).to_broadcast([128, NCK, 128]), ALU.subtract),
               reads=[r["mm"]], writes=[rl])
            for s0 in range(0, S, 2048):
                n = min(2048, S - s0)
                op("act", lambda a: a.activation(e1[:, 0:n], lf[:, s0:s0 + n], AF.Exp, scale=-1.0), reads=[rl], writes=[r["e1"]])
                op("pool", lambda g: g.tensor_tensor(khat[:, s0:s0 + n], kf[:, s0:s0 + n], e1[:, 0:n], ALU.mult),
                   reads=[rk, r["e1"]], writes=[r["khat"]])
            for s0 in range(0, O, 2048):
                n = min(2048, O - s0)
                op("act", lambda a: a.activation(e1[:, 0:n], lf[:, P + s0:P + s0 + n], AF.Exp), reads=[rl], writes=[r["e1"]])
                op("dve", lambda v: v.scalar_tensor_tensor(qhat[:, s0:s0 + n], qs[:, s0:s0 + n], 128.0 ** -0.5, e1[:, 0:n], ALU.mult, ALU.mult),
                   reads=[rq, r["e1"]], writes=[r["qhat"]])
            for c0 in range(0, NCK, 4):
                pt, rp = rot_ptb()
                for i in range(4):
                    c = c0 + i
                    op("pe", lambda p: p.transpose(pt[:, i, :], khat[:, c * 128:(c + 1) * 128], self.identb[:]),
                       reads=[r["khat"], cr], writes=[rp])
                op("act", lambda a: a.copy(ktok[:, c0:c0 + 4, :], pt[:]), reads=[rp], writes=[r["ktok"]])
            for c in range(NCK - 1):
                pk, rpk = rot_pb()
                op("pe", lambda p: p.matmul(pk[:, 0:128], ktok[:, c, :], vtok[:, c, :], start=True, stop=True),
                   reads=[r["ktok"], rv], writes=[rpk])
                op("act", lambda a: a.activation(kvs[:, c, :], pk[:, 0:128], AF.Copy, scale=elm[:, c:c + 1]),
                   reads=[rpk, r["elm"]], writes=[rkv[c]])
            for co in range(OC):
                c = PC + co
                psc, rsc = rot_pb()
                op("pe", lambda p: p.matmul(psc[:, 0:128], khat[:, c * 128:(c + 1) * 128], qhat[:, co * 128:(co + 1) * 128], start=True, stop=True),
                   reads=[r["khat"], r["qhat"]], writes=[rsc])
                op("dve", lambda v: v.tensor_tensor(scmall[:, co, :], psc[:, 0:128], triu[:], ALU.mult), reads=[rsc, rB], writes=[rscm[co]])
            op("dve", lambda v: v.memset(Sf[:], 0.0), writes=[r["Sf"]])
            for c in range(NCK):
                co = c - PC
                if co >= 0:
                    op("dve", lambda v: v.tensor_scalar(sball[:, co, :], Sf[:], em[:, c:c + 1], None, ALU.mult),
                       reads=[r["Sf"], r["em"]], writes=[rsb[co]])
                if c < NCK - 1:
                    op("dve", lambda v: v.scalar_tensor_tensor(Sf[:], Sf[:], el[:, c:c + 1], kvs[:, c, :], ALU.mult, ALU.add),
                       reads=[rkv[c], r["el"]], writes=[r["Sf"]])
            for co in range(OC):
                c = PC + co
                po, rpo = rot_pb()
                op("pe", lambda p: p.matmul(po[:, 0:128], vtok[:, c, :], scmall[:, co, :], start=True, stop=False),
                   reads=[rv, rscm[co]], writes=[rpo])
                op("pe", lambda p: p.matmul(po[:, 0:128], sball[:, co, :], qhat[:, co * 128:(co + 1) * 128], start=False, stop=True),
                   reads=[rsb[co], r["qhat"]], writes=[rpo])
                op("act", lambda a: a.copy(oall[:, co * 128:(co + 1) * 128], po[:, 0:128]), reads=[rpo], writes=[r["oall"]])
            op("act", lambda a: a.activation(sqo[:], oall[:], AF.Square), reads=[r["oall"]], writes=[r["sqo"]])
            for s0 in range(0, O, 512):
                pss, rss = rot_pb()
                op("pe", lambda p: p.matmul(pss[:], self.onesb[:], sqo[:, s0:s0 + 512], start=True, stop=True),
                   reads=[r["sqo"], cr], writes=[rss])
                op("act", lambda a: a.activation(rst[:, s0:s0 + 512], pss[:], AF.Sqrt, bias=EPS, scale=1.0 / 128), reads=[rss], writes=[r["rst"]])
            op("dve", lambda v: v.reciprocal(rst[:], rst[:]), reads=[r["rst"]], writes=[r["rst"]])
            op("dve", lambda v: v.scalar_tensor_tensor(oall[:], oall[:], self.hgain[:, 0:1], rst[:], ALU.mult, ALU.mult),
               reads=[r["rst"], cr], writes=[r["oall"]])
            op("pool", lambda g: g.tensor_tensor(ofin[:], oall[:], gg[:], ALU.mult), reads=[r["oall"], rg], writes=[r["ofin"]])
            dma("sp", self.OM[h], ofin[:], r["ofin"], reads=[r["ofin"]])
        self.barrier()
        es.close()

    def phase_b_moba(self):
        cfg, nc = self.cfg, self.nc
        NH, S, O, P, NCK, NB, NQT = cfg.NH, cfg.S, cfg.O, cfg.P, cfg.NCK, cfg.NB, cfg.NQT
        es = ExitStack()
        sb = lambda *a: es.enter_context(nc.sbuf_tensor(*a))
        ps = lambda *a: es.enter_context(nc.psum_tensor(*a))
        op, dma = self.op, self.dma
        cr = self.r_const
        pmask = sb("pmask_sb", [128, NQT, NB], F32)
        iota = sb("iota", [128, NB], F32)
        rB = Res("constM")
        dma("sp", iota[:], self.c_iota, rB, writes=[rB])
        with nc.allow_non_contiguous_dma(reason="tiny"):
            dma("sp", pmask[:], self.pmask_d.rearrange("q p n -> p q n"), rB, writes=[rB])
        NSTR = 2 if NH >= 2 else 1
        pb = [ps("mpb%d" % i, [128, 512], F32) for i in range(2)]
        rpb = [Res("mpb%d" % i) for i in range(2)]
        ptb = [ps("mptb%d" % i, [128, 4, 128], BF16) for i in range(2)]
        rptb = [Res("mptb%d" % i) for i in range(2)]
        st = {"pb": 0, "ptb": 0}

        def rot_pb():
            i = st["pb"]; st["pb"] = (i + 1) % 2
            return pb[i], rpb[i]

        def rot_ptb():
            i = st["ptb"]; st["ptb"] = (i + 1) % 2
            return ptb[i], rptb[i]

        scale = 128.0 ** -0.5
        NS = 3

        def make_stream(si):
            z = "s%d" % si
            d = {}
            d["pacc"] = [ps("pacc%s_%d" % (z, i), [128, 512], F32) for i in range(2)]
            d["rpacc"] = [Res("pacc") for i in range(2)]
            d["inb"] = []
            for i in range(2):
                e = dict(qT=sb("qT%s_%d" % (z, i), [128, O], BF16), kT=sb("kT%s_%d" % (z, i), [128, S], BF16),
                         mv=sb("mv%s_%d" % (z, i), [128, NCK, 129], BF16), akr=sb("akr%s_%d" % (z, i), [128, 256], BF16),
                         akc=sb("akc%s_%d" % (z, i), [128, 2, 2, 256], BF16))
                e["r"] = {k: Res(k) for k in ("qT", "kT", "mv", "akr", "akc")}
                op("pool", lambda g, e=e: g.memset(e["mv"][:, :, 128:129], 1.0), writes=[e["r"]["mv"]])
                d["inb"].append(e)
            for n_, shp, dt in [("km", [128, NB], F32), ("kmb", [128, NB], BF16), ("gm", [128, NB], F32), ("top8", [128, 8], F32),
                                ("b1", [128, NB], F32), ("rb", [128, NQT, NB], F32), ("tq", [128, NQT, NB], F32),
                                ("rsum", [128, NQT, NB + 1], F32), ("lsum", [128, 1], F32), ("on", [128, 128], BF16)]:
                d[n_] = sb(n_ + z, shp, dt)
            d["ssb"] = [sb("ssb%s_%d" % (z, i), [128, 256], F32) for i in range(NS)]
            d["pbf"] = [sb("pbf%s_%d" % (z, i), [128, 256], BF16) for i in range(NS)]
            d["pT"] = [sb("pT%s_%d" % (z, i), [128, 2, 128], BF16) for i in range(NS)]
            d["rssb"] = [Res("ssb") for i in range(NS)]
            d["rpbf"] = [Res("pbf") for i in range(NS)]
            d["rpT"] = [Res("pT") for i in range(NS)]
            d["rrs"] = [Res("rsum") for q in range(NQT)]
            d["oT"] = [sb("oT%s_%d" % (z, i), [128, O], BF16) for i in range(2)]
            d["roT"] = [Res("oT") for i in range(2)]
            d["r"] = {n_: Res(n_) for n_ in ["km", "kmb", "gm", "top8", "b1", "lsum", "on"]}
            d["rrb"] = [Res("rb") for q in range(NQT)]
            d["rtq"] = [Res("tq") for q in range(NQT)]
            return d

        def loads(d, h, slot):
            b = d["inb"][slot]
            dma("sp", b["qT"][:], self.MQ[h], b["r"]["qT"], writes=[b["r"]["qT"]])
            dma("sp", b["kT"][:], self.MK[h], b["r"]["kT"], writes=[b["r"]["kT"]])
            with nc.allow_non_contiguous_dma(reason="256B rows"):
                dma("sp", b["mv"][:, :, 0:128], self.MV[h].rearrange("c p d -> p c d"), b["r"]["mv"], writes=[b["r"]["mv"]])
            dma("sp", b["akr"][:], self.c_akr[h], b["r"]["akr"], writes=[b["r"]["akr"]])
            dma("sp", b["akc"][:], self.c_akc[h].rearrange("a b p j -> p a b j"), b["r"]["akc"], writes=[b["r"]["akc"]])

        def stream_gen(d, heads):
            r = d["r"]
            km, kmb, gm, top8, b1, rb, tq, rsum, lsum, on = (d[k] for k in ("km", "kmb", "gm", "top8", "b1", "rb", "tq", "rsum", "lsum", "on"))
            ssb, pbf, pT, rssb, rpbf, rpT, rrs, rrb, rtq = (d[k] for k in ("ssb", "pbf", "pT", "rssb", "rpbf", "rpT", "rrs", "rrb", "rtq"))
            loads(d, heads[0], 0)
            for hi, h in enumerate(heads):
                if hi + 1 < len(heads):
                    loads(d, heads[hi + 1], (hi + 1) % 2)
                b = d["inb"][hi % 2]
                qT, kT, mv, akr, akc = b["qT"], b["kT"], b["mv"], b["akr"], b["akc"]
                rq, rk, rv, rakr, rakc = (b["r"][k] for k in ("qT", "kT", "mv", "akr", "akc"))
                oTh, roTh = d["oT"][hi % 2], d["roT"][hi % 2]
                slope = float(np.exp2(np.float32(-8.0) * np.float32(h + 1) / np.float32(NH)))
                op("dve", lambda v: v.tensor_reduce(km[:], kT[:].rearrange("p (n l) -> p n l", l=256), AX.X, ALU.add),
                   reads=[rk], writes=[r["km"]])
                op("dve", lambda v: v.tensor_scalar(kmb[:], km[:], 1.0 / 256, None, ALU.mult), reads=[r["km"]], writes=[r["kmb"]])
                yield
                info = []
                for qt in range(NQT):
                    qq = qT[:, qt * 128:(qt + 1) * 128]
                    tpos = P + qt * 128
                    ob = tpos // 256
                    half = (tpos % 256) // 128
                    info.append((qq, ob, half))
                    pg, rpg = rot_pb()
                    op("pe", lambda p: p.matmul(pg[:, 0:NB], qq, kmb[:], start=True, stop=True), reads=[rq, r["kmb"]], writes=[rpg])
                    op("dve", lambda v: v.tensor_tensor(gm[:], pg[:, 0:NB], pmask[:, qt, :], ALU.add), reads=[rpg, rB], writes=[r["gm"]])
                    op("dve", lambda v: v.max(top8[:], gm[:]), reads=[r["gm"]], writes=[r["top8"]])
                    op("dve", lambda v: v.tensor_scalar(b1[:], gm[:], top8[:, 2:3], None, ALU.is_ge), reads=[r["gm"], r["top8"]], writes=[r["b1"]])
                    op("dve", lambda v: v.scalar_tensor_tensor(b1[:], gm[:], NEG / 2, b1[:], ALU.is_gt, ALU.mult), reads=[r["gm"]], writes=[r["b1"]])
                    op("dve", lambda v: v.tensor_scalar(tq[:, qt, :], iota[:], float(tpos), -slope, ALU.add, ALU.mult), reads=[rB], writes=[rtq[qt]])
                    op("dve", lambda v: v.tensor_scalar(rb[:, qt, :], b1[:], -1.0, -NEG, ALU.add, ALU.mult), reads=[r["b1"]], writes=[rrb[qt]])
                    op("dve", lambda v: v.tensor_tensor(rb[:, qt, :], rb[:, qt, :], tq[:, qt, :], ALU.add), reads=[rtq[qt]], writes=[rrb[qt]])
                    yield
                pairs = [(qt, n) for qt in range(NQT) for n in range(info[qt][1] + 1)]

                def stA(k):
                    qt, n = pairs[k]
                    qq, ob, half = info[qt]
                    i3 = k % NS
                    pss, rss = rot_pb()
                    op("pe", lambda p: p.matmul(pss[:, 0:256], qq, kT[:, n * 256:(n + 1) * 256], start=True, stop=False),
                       reads=[rq, rk], writes=[rss])
                    if n < ob:
                        op("pe", lambda p: p.matmul(pss[:, 0:256], self.onesb[:], akr[:], start=False, stop=True),
                           reads=[rakr, cr], writes=[rss])
                        bias, rbias = rb[:, qt, n:n + 1], rrb[qt]
                    else:
                        op("pe", lambda p: p.matmul(pss[:, 0:256], self.identb[:], akc[:, half, 0, :], start=False, stop=False),
                           reads=[rakc, cr], writes=[rss])
                        op("pe", lambda p: p.matmul(pss[:, 0:256], self.identb[:], akc[:, half, 1, :], start=False, stop=True),
                           reads=[rakc, cr], writes=[rss])
                        bias, rbias = tq[:, qt, n:n + 1], rtq[qt]
                    op("act", lambda a_: a_.activation(pbf[i3][:], pss[:, 0:256], AF.Exp, bias=bias, scale=scale),
                       reads=[rss, rbias], writes=[rpbf[i3]])

                def stB(k):
                    i3 = k % NS
                    pt, rp = rot_ptb()
                    for hf in range(2):
                        op("pe", lambda p: p.transpose(pt[:, hf, :], pbf[i3][:, hf * 128:(hf + 1) * 128], self.identb[:]),
                           reads=[rpbf[i3], cr], writes=[rp])
                    if k % 4 == 3:
                        op("act", lambda a_: a_.copy(pT[i3][:], pt[:, 0:2, :]), reads=[rp], writes=[rpT[i3]])
                    else:
                        op("dve", lambda v: v.tensor_copy(pT[i3][:], pt[:, 0:2, :]), reads=[rp], writes=[rpT[i3]])

                def stC(k):
                    qt, n = pairs[k]
                    qq, ob, half = info[qt]
                    i3 = k % NS
                    po, rpo = d["pacc"][qt % 2], d["rpacc"][qt % 2]
                    for hf in range(2):
                        op("pe", lambda p: p.matmul(po[:, 0:129], pT[i3][:, hf, :], mv[:, 2 * n + hf, :],
                                                    start=(n == 0 and hf == 0), stop=(n == ob and hf == 1)),
                           reads=[rpT[i3], rv], writes=[rpo])
                    if n == ob:
                        op("dve", lambda v: v.reciprocal(lsum[:], po[:, 128:129]), reads=[rpo], writes=[r["lsum"]])
                        op("dve", lambda v: v.tensor_scalar(on[:], po[:, 0:128], lsum[:, 0:1], None, ALU.mult), reads=[rpo, r["lsum"]], writes=[r["on"]])
                        pt, rp = rot_ptb()
                        op("pe", lambda p: p.transpose(pt[:, 0, :], on[:], self.identb[:]), reads=[r["on"], cr], writes=[rp])
                        op("act", lambda a_: a_.copy(oTh[:, qt * 128:(qt + 1) * 128], pt[:, 0, :]), reads=[rp], writes=[roTh])

                for step in range(len(pairs) + 2):
                    if step < len(pairs):
                        stA(step)
                    if 0 <= step - 1 < len(pairs):
                        stB(step - 1)
                    if 0 <= step - 2 < len(pairs):
                        stC(step - 2)
                    yield
                dma("sp", self.OM[NH + h], oTh[:], roTh, reads=[roTh])

        jobs = self.conv_jobs_c()
        jobs = jobs[:int(len(jobs) * cfg.conv_frac)]
        ngmax = max(cfg.groups)
        cst = {"wk": [sb("cwk%d" % i, [128, cfg.DC, 128], BF16) for i in range(3)],
               "wd": [sb("cwd%d" % i, [128, ngmax, 128], BF16) for i in range(2)]}
        rcst = {k: [Res("cst") for _ in v] for k, v in cst.items()}
        cnt = {"wk": 0, "wd": 0}

        def conv_step():
            if not jobs:
                return
            kind, shape, key, src_ap, ng = jobs.pop(0)
            cache_ap, cres = self.cache_for(kind, shape, key)
            if key[1] in cres:
                return
            i = cnt[kind] % len(cst[kind]); cnt[kind] += 1
            t, rt = cst[kind][i], rcst[kind][i]
            dst = t[:] if ng is None else t[:, 0:ng, :]
            dma("pool", dst, src_ap, rt, writes=[rt])
            cres[key[1]] = Res("wc")
            dma("sp", cache_ap, t[:], rt, reads=[rt], writes=[cres[key[1]]])

        gens = [stream_gen(make_stream(si), list(range(si, NH, NSTR))) for si in range(NSTR)]
        active = list(gens)
        it = 0
        while active:
            for g in list(active):
                try:
                    next(g)
                except StopIteration:
                    active.remove(g)
            it += 1
            if it % cfg.conv_every == 0:
                conv_step()
        self.barrier()
        es.close()


def make_consts(cfg):
    NH = cfg.NH
    c = {}
    c["c_identf"] = np.eye(128, dtype=np.float32)
    c["c_identb"] = np.eye(128, dtype=np.float32).astype(ml_dtypes.bfloat16)
    c["c_onesb"] = np.ones((128, 128), np.float32).astype(ml_dtypes.bfloat16)
    c["c_triu"] = np.triu(np.ones((128, 128), np.float32))
    sm = np.ones((128, 2048), np.float32)
    sm[:, ::128] = 0.0
    c["c_scanm"] = sm
    scale = np.float32(128.0 ** -0.5)
    j = np.arange(256, dtype=np.float32)[None, :]
    i = np.arange(128, dtype=np.float32)[:, None]
    bf = ml_dtypes.bfloat16

    def hilo(a):
        hi = a.astype(bf)
        lo = (a - hi.astype(np.float32)).astype(bf)
        return hi, lo

    akr = np.zeros((NH, 128, 256), bf)
    akc = np.zeros((NH, 2, 2, 128, 256), bf)
    for h in range(NH):
        slope = np.exp2(np.float32(-8.0) * np.float32(h + 1) / np.float32(NH)).astype(np.float32)
        base = (slope * j / scale).astype(np.float32)
        akr[h, 0], akr[h, 1] = (a[0] for a in hilo(base))
        for half in range(2):
            m = np.where(j <= i + 128 * half, np.broadcast_to(base, (128, 256)), np.float32(NEG) / scale).astype(np.float32)
            akc[h, half, 0], akc[h, half, 1] = hilo(m)
    c["c_akr"] = akr
    c["c_akc"] = akc
    c["c_iota"] = (i - 256.0 * np.arange(cfg.NB, dtype=np.float32)[None, :]).astype(np.float32)
    return c


def make_pmask(cfg, has_prefix):
    pm = np.full((cfg.NQT, 128, cfg.NB), NEG, np.float32)
    first = 0 if has_prefix else cfg.P // 256
    for qt in range(cfg.NQT):
        ob = (cfg.P + qt * 128) // 256
        pm[qt, :, first:ob] = 0.0
    return pm


_CACHE = {}


def run(cfg, inputs, n_batch, trace=False):
    if "nc" not in _CACHE or _CACHE.get("key") != (cfg.D, cfg.FF, cfg.NH, cfg.P, cfg.O, cfg.debug):
        _CACHE["nc"] = Builder(cfg).build()
        _CACHE["key"] = (cfg.D, cfg.FF, cfg.NH, cfg.P, cfg.O, cfg.debug)
    nc = _CACHE["nc"]
    f32 = lambda a: np.ascontiguousarray(np.asarray(a, dtype=np.float32))
    x = f32(inputs["x"])
    shared = dict(make_consts(cfg))
    for f in ("ffn1", "ffn2"):
        for w in ("w_gate", "w_up", "w_down"):
            shared[f + "_" + w] = f32(inputs[f + "_" + w][0])
    shared["w_in"] = f32(inputs["w_in"][0])
    shared["w_out"] = f32(inputs["w_out"][0])
    shared["gains"] = np.stack([f32(inputs["ffn1_norm"][0]), f32(inputs["mix_norm"][0]),
                                f32(inputs["ffn2_norm"][0]), f32(inputs["final_norm"])])
    shared["hgrn_out_norm"] = f32(inputs["hgrn_out_norm"][0]).reshape(128, 1)
    shared["hgrn_lower_bounds"] = f32(inputs["hgrn_lower_bounds"])
    pm = [make_pmask(cfg, False), make_pmask(cfg, True)]
    in_maps = []
    for c in range(2 * n_batch):
        b, s = c // 2, c % 2
        xc = np.zeros((cfg.S, cfg.D), np.float32)
        if s == 0:
            xc[cfg.P:] = x[b, :cfg.O]
        else:
            xc[:] = x[b]
        m = dict(shared)
        m["x"] = xc
        m["pmask"] = pm[s]
        in_maps.append(m)
    res = run_bass_kernel_spmd(nc, in_maps, core_ids=list(range(2 * n_batch)), trace=trace)
    out = np.zeros((n_batch, 2 * cfg.O, cfg.D), np.float32)
    for c in range(2 * n_batch):
        b, s = c // 2, c % 2
        out[b, s * cfg.O:(s + 1) * cfg.O] = res.results[c]["y"]
    return out, res


def kernel(**inputs):
    cfg = Cfg()
    out, _ = run(cfg, inputs, 4)
    return out
```
